# Optimizing a Trainium2 kernel written in Bass

```python
import jax, jax.numpy as jnp
from jax import lax
import numpy as np

D_MODEL = 1024
BATCH = 8
SEQ = 2048
DEPTH = 1

MIX_WIDTH = D_MODEL
HG_WIDTH = MIX_WIDTH // 2
HG_HEADS = 4
HG_DIM = HG_WIDTH // HG_HEADS
ATT_WIDTH = MIX_WIDTH - HG_WIDTH
ATT_HEADS = 8
ATT_DIM = ATT_WIDTH // ATT_HEADS
BLOCK = 256
TOPK = 3
Q_CHUNK = 32
HG_CHUNK = 64
D_FF = -(-8 * D_MODEL // (3 * 256)) * 256
IN_COLS = 4 * HG_WIDTH + 3 * ATT_WIDTH
EPS = 1e-6

kernel_name = "hymba_hgrn2_moba_alibi_adaln_block"


def rms_norm(t, gain):
    tf = t.astype(jnp.float32)
    tf = tf * lax.rsqrt(jnp.mean(tf * tf, axis=-1, keepdims=True) + EPS)
    return (tf * gain.astype(jnp.float32)).astype(t.dtype)


def split_heads(t, n):
    B, T, _ = t.shape
    return t.reshape(B, T, n, -1).transpose(0, 2, 1, 3)


def merge_heads(t):
    B, H, T, d = t.shape
    return t.transpose(0, 2, 1, 3).reshape(B, T, H * d)


def hgrn2_mixer(q, f_logit, i, g, lb, out_gain):
    B, H, T, d = q.shape
    f32 = jnp.float32
    lbb = lb[None, :, None, :]
    f = lbb + (1.0 - lbb) * jax.nn.sigmoid(f_logit.astype(f32))
    log_f = jnp.log(f)
    k = 1.0 - f
    qf = jax.nn.silu(q.astype(f32))
    v = i.astype(f32)
    nc = T // HG_CHUNK

    def chunks(t):
        return t.reshape(B, H, nc, HG_CHUNK, d).transpose(2, 0, 1, 3, 4)

    tri = jnp.tril(jnp.ones((HG_CHUNK, HG_CHUNK), dtype=bool))[None, None, :, :, None]

    def step(S, inp):
        qc, kc, vc, lfc = inp
        b = jnp.cumsum(lfc, axis=2)
        diff = b[:, :, :, None, :] - b[:, :, None, :, :]
        decay = jnp.where(tri, jnp.exp(jnp.where(tri, diff, 0.0)), 0.0)
        attn = jnp.einsum('bhtsk,bhsk->bhts', decay * qc[:, :, :, None, :], kc)
        o = jnp.einsum('bhts,bhsv->bhtv', attn, vc) + jnp.einsum('bhtk,bhkv->bhtv', qc * jnp.exp(b), S)
        b_last = b[:, :, -1:, :]
        S = jnp.exp(b_last[:, :, 0, :, None]) * S + jnp.einsum('bhsk,bhsv->bhkv', kc * jnp.exp(b_last - b), vc)
        return S, o

    S0 = jnp.zeros((B, H, d, d), f32)
    _, o = lax.scan(step, S0, (chunks(qf), chunks(k), chunks(v), chunks(log_f)))
    o = o.transpose(1, 2, 0, 3, 4).reshape(B, H, T, d)
    o = rms_norm(o, out_gain) * jax.nn.silu(g.astype(f32))
    return o.astype(q.dtype)


def moba_mixer(q, k, v, q_gain, k_gain):
    B, H, T, dh = q.shape
    f32 = jnp.float32
    q = rms_norm(q, q_gain)
    k = rms_norm(k, k_gain)
    nb = -(-T // BLOCK)
    pad = nb * BLOCK - T
    kp = jnp.pad(k, ((0, 0), (0, 0), (0, pad), (0, 0)))
    vp = jnp.pad(v, ((0, 0), (0, 0), (0, pad), (0, 0)))
    kb = kp.reshape(B, H, nb, BLOCK, dh)
    vb = vp.reshape(B, H, nb, BLOCK, dh)

    k_mean = jnp.mean(kb.astype(f32), axis=3)
    gate = jnp.einsum('bhtd,bhnd->bhtn', q.astype(f32), k_mean)
    pos = jnp.arange(T)
    qblk = pos // BLOCK
    past = jnp.arange(nb)[None, :] < qblk[:, None]
    gate = jnp.where(past[None, None], gate, -jnp.inf)
    n_sel = min(TOPK, nb)
    _, idx = lax.top_k(gate, n_sel)
    sel_valid = jnp.arange(n_sel)[None, :] < qblk[:, None]

    slopes = jnp.exp2(-8.0 * jnp.arange(1, H + 1, dtype=f32) / H)
    scale = dh ** -0.5
    bi = jnp.arange(B)[:, None, None, None]
    hi = jnp.arange(H)[None, :, None, None]
    offs = jnp.arange(BLOCK)

    def chunk_fn(cidx):
        t0 = cidx * Q_CHUNK
        qc = lax.dynamic_slice_in_dim(q, t0, Q_CHUNK, axis=2)
        ic = lax.dynamic_slice_in_dim(idx, t0, Q_CHUNK, axis=2)
        vmask = lax.dynamic_slice_in_dim(sel_valid, t0, Q_CHUNK, axis=0)
        tq = (t0 + jnp.arange(Q_CHUNK)).astype(f32)
        kg = kb[bi, hi, ic]
        vg = vb[bi, hi, ic]
        sp = (ic[..., None] * BLOCK + offs).astype(f32)
        s_past = jnp.einsum('bhqd,bhqnkd->bhqnk', qc, kg).astype(f32) * scale
        s_past = s_past - slopes[None, :, None, None, None] * (tq[None, None, :, None, None] - sp)
        s_past = jnp.where(vmask[None, None, :, :, None], s_past, -jnp.inf)
        j0 = (t0 // BLOCK) * BLOCK
        ko = lax.dynamic_slice_in_dim(kp, j0, BLOCK, axis=2)
        vo = lax.dynamic_slice_in_dim(vp, j0, BLOCK, axis=2)
        so = (j0 + offs).astype(f32)
        s_own = jnp.einsum('bhqd,bhkd->bhqk', qc, ko).astype(f32) * scale
        s_own = s_own - slopes[None, :, None, None] * (tq[:, None] - so[None, :])[None, None]
        s_own = jnp.where((so[None, :] <= tq[:, None])[None, None], s_own, -jnp.inf)
        logits = jnp.concatenate([s_past.reshape(B, H, Q_CHUNK, n_sel * BLOCK), s_own], axis=-1)
        p = jax.nn.softmax(logits, axis=-1).astype(v.dtype)
        p_past = p[..., :n_sel * BLOCK].reshape(B, H, Q_CHUNK, n_sel, BLOCK)
        p_own = p[..., n_sel * BLOCK:]
        return (jnp.einsum('bhqnk,bhqnkd->bhqd', p_past, vg)
                + jnp.einsum('bhqk,bhkd->bhqd', p_own, vo))

    out = lax.map(chunk_fn, jnp.arange(T // Q_CHUNK))
    return out.transpose(1, 2, 0, 3, 4).reshape(B, H, T, dh)


def setup_inputs(seed: int = 0) -> dict:
    key = jax.random.key(seed)
    ks = jax.random.split(key, 20)
    f32 = jnp.float32

    def w(k, shape, fan_in, mult=1.0):
        return jax.random.normal(k, shape, f32) * (mult * fan_in ** -0.5)

    def gain(k, shape):
        return 1.0 + 0.05 * jax.random.normal(k, shape, f32)

    return {
        "x": jax.random.normal(ks[0], (BATCH, SEQ, D_MODEL), f32),
        "c": jax.random.normal(ks[1], (BATCH, D_MODEL), f32),
        "w_ada": w(ks[2], (DEPTH, D_MODEL, 6 * D_MODEL), D_MODEL, 0.5),
        "b_ada": 0.02 * jax.random.normal(ks[3], (DEPTH, 6 * D_MODEL), f32),
        "norm1_g": gain(ks[4], (DEPTH, D_MODEL)),
        "w_in": w(ks[5], (DEPTH, D_MODEL, IN_COLS), D_MODEL),
        "lb_logits": 0.5 * jax.random.normal(ks[6], (DEPTH + 1, HG_WIDTH), f32),
        "hg_norm_g": gain(ks[7], (DEPTH, HG_DIM)),
        "q_norm_g": gain(ks[8], (DEPTH, ATT_DIM)),
        "k_norm_g": gain(ks[9], (DEPTH, ATT_DIM)),
        "w_out": w(ks[10], (DEPTH, MIX_WIDTH, D_MODEL), MIX_WIDTH),
        "norm2_g": gain(ks[11], (DEPTH, D_MODEL)),
        "w_gate": w(ks[12], (DEPTH, D_MODEL, D_FF), D_MODEL),
        "w_up": w(ks[13], (DEPTH, D_MODEL, D_FF), D_MODEL),
        "w_down": w(ks[14], (DEPTH, D_FF, D_MODEL), D_FF),
    }


def reference(x, c, w_ada, b_ada, norm1_g, w_in, lb_logits, hg_norm_g, q_norm_g, k_norm_g,
              w_out, norm2_g, w_gate, w_up, w_down):
    lbs = jnp.cumsum(jax.nn.softmax(lb_logits.astype(jnp.float32), axis=0), axis=0)
    c_act = jax.nn.silu(c)
    for l in range(DEPTH):
        mod = c_act @ w_ada[l] + b_ada[l]
        shift1, scale1, gate1, shift2, scale2, gate2 = [m[:, None, :] for m in jnp.split(mod, 6, axis=-1)]

        h = rms_norm(x, norm1_g[l]) * (1.0 + scale1) + shift1
        proj = h @ w_in[l]
        hq, hf, hi_, hg, aq, ak, av = jnp.split(
            proj, np.cumsum([HG_WIDTH] * 4 + [ATT_WIDTH] * 2).tolist(), axis=-1)
        lb = lbs[l].reshape(HG_HEADS, HG_DIM)
        o_hg = hgrn2_mixer(split_heads(hq, HG_HEADS), split_heads(hf, HG_HEADS),
                           split_heads(hi_, HG_HEADS), split_heads(hg, HG_HEADS),
                           lb, hg_norm_g[l])
        o_att = moba_mixer(split_heads(aq, ATT_HEADS), split_heads(ak, ATT_HEADS),
                           split_heads(av, ATT_HEADS), q_norm_g[l], k_norm_g[l])
        mixed = jnp.concatenate([merge_heads(o_hg), merge_heads(o_att)], axis=-1)
        x = x + gate1 * (mixed @ w_out[l])

        h2 = rms_norm(x, norm2_g[l]) * (1.0 + scale2) + shift2
        ffn = (jax.nn.silu(h2 @ w_gate[l]) * (h2 @ w_up[l])) @ w_down[l]
        x = x + gate2 * ffn
    return x
```

```python
import contextlib
import os
import numpy as np
import ml_dtypes
import concourse.bass as bass
import concourse.mybir as mybir
from concourse.bass_utils import run_bass_kernel_spmd

F32 = mybir.dt.float32
BF16 = mybir.dt.bfloat16
ALU = mybir.AluOpType
AF = mybir.ActivationFunctionType
AX = mybir.AxisListType

T = 2048
D = 1024
NT = 16
DFF = 2816
NFF = 22
EPS = 1e-6
BIG = 30000.0
NEG = -1.0e30
ENGS = ("pe", "act", "dve", "pool", "sp")
DEBUG = os.environ.get("KDEBUG", "")


class Sched:
    def __init__(self, nc, stack):
        self.nc = nc
        self.ops = {e: [] for e in ENGS}
        self.cnt = {e: 0 for e in ENGS}
        self.nops = {e: 0 for e in ENGS}
        self.waited = {e: {} for e in ENGS}
        self.res = {}
        self.esem = {e: stack.enter_context(nc.semaphore("c_" + e)) for e in ENGS if e != "sp"}
        self.dsem = {}
        self.dcnt = {}
        self.stack = stack
        self.pending = {e: [] for e in ENGS}

    def _dma_sem(self, key):
        if key not in self.dsem:
            self.dsem[key] = self.stack.enter_context(self.nc.semaphore("d_%d" % len(self.dsem)))
            self.dcnt[key] = 0
        return self.dsem[key]

    def _add(self, eng, waits, sem, val):
        if self.waited[eng].get(id(sem), 0) >= val:
            return
        old = waits.get(id(sem))
        if old is None or old[1] < val:
            waits[id(sem)] = (sem, val)

    def _need(self, eng, waits, tok):
        if tok is None:
            return
        sem, val, peng, pidx = tok
        if peng == eng:
            return
        self._add(eng, waits, sem, val)

    def _deps(self, eng, reads, writes):
        waits = {}
        my_idx = self.nops[eng]
        for k in reads:
            r = self.res.get(k)
            if r and r[0] is not None:
                tok = r[0]
                self._need(eng, waits, tok)
                if tok[2] == eng and eng in ("act", "dve", "pool"):
                    self._add(eng, waits, tok[0], tok[1])
        for k in writes:
            r = self.res.get(k)
            if r:
                self._need(eng, waits, r[0])
                for tok in r[1]:
                    self._need(eng, waits, tok)
                if eng in ("act", "dve", "pool"):
                    for tok in ([r[0]] if r[0] is not None else []) + r[1]:
                        if tok[2] == eng:
                            self._add(eng, waits, tok[0], tok[1])
        out = []
        for sid, (sem, val) in waits.items():
            self.waited[eng][sid] = val
            out.append((sem, val))
        return out

    def _commit(self, reads, writes, tok):
        for k in reads:
            r = self.res.setdefault(k, [None, []])
            r[1].append(tok)
        for k in writes:
            self.res[k] = [tok, []]

    def op(self, eng, fn, reads=(), writes=(), signal=True):
        waits = self._deps(eng, reads, writes)
        idx = self.nops[eng]
        self.nops[eng] += 1
        if signal:
            self.cnt[eng] += 1
            tok = (self.esem[eng], self.cnt[eng], eng, idx)
            self.ops[eng].append((waits, fn, (self.esem[eng], 1)))
            for (rr, ww) in self.pending[eng]:
                self._commit(rr, ww, tok)
            self.pending[eng] = []
            self._commit(reads, writes, tok)
        else:
            self.ops[eng].append((waits, fn, None))
            self.pending[eng].append((list(reads), list(writes)))

    def dma(self, eng, out, in_, reads=(), writes=(), semkey=None):
        waits = self._deps(eng, reads, writes)
        sem = self._dma_sem(semkey)
        self.dcnt[semkey] += 16
        tok = (sem, self.dcnt[semkey], "dma", -1)
        self.nops[eng] += 1
        self.ops[eng].append((waits, (lambda e: e.dma_start(out=out, in_=in_)), (sem, 16)))
        self._commit(reads, writes, tok)

    def barrier(self, engs=("pe", "act", "dve", "sp")):
        for e in engs:
            waits = {}
            for o in ("pe", "act", "dve"):
                if o != e and self.cnt[o] > 0:
                    self._add(e, waits, self.esem[o], self.cnt[o])
            lst = []
            for sid, (sem, val) in waits.items():
                self.waited[e][sid] = val
                lst.append((sem, val))
            self.ops[e].append((lst, None, None))

    def fence(self, engs, keys):
        for e in engs:
            waits = self._deps(e, (), keys)
            self.ops[e].append((waits, None, None))

    def wait_all(self, eng, keys):
        waits = self._deps(eng, keys, ())
        self.ops[eng].append((waits, None, None))

    def emit(self):
        nc = self.nc
        with nc.Block() as block:
            def mk(ename):
                def body(e):
                    for waits, fn, inc in self.ops[ename]:
                        for sem, val in waits:
                            e.wait_ge(sem, val)
                        if fn is None:
                            continue
                        ins = fn(e)
                        if inc is not None:
                            ins.then_inc(inc[0], inc[1])
                return body
            block.tensor(mk("pe"))
            block.scalar(mk("act"))
            block.vector(mk("dve"))
            block.gpsimd(mk("pool"))
            block.sync(mk("sp"))


def MM(out, lhsT, rhs, start=True, stop=True):
    return lambda e: e.matmul(out, lhsT=lhsT, rhs=rhs, start=start, stop=stop)


def TR(out, in_, ident):
    return lambda e: e.transpose(out, in_, ident)


def ACTF(out, in_, func, **kw):
    return lambda e: e.activation(out=out, in_=in_, func=func, **kw)


def TT(out, in0, in1, op):
    return lambda e: e.tensor_tensor(out=out, in0=in0, in1=in1, op=op)


def TS(out, in0, s1, s2, op0, op1=None):
    if op1 is None:
        return lambda e: e.tensor_scalar(out=out, in0=in0, scalar1=s1, scalar2=None, op0=op0)
    return lambda e: e.tensor_scalar(out=out, in0=in0, scalar1=s1, scalar2=s2, op0=op0, op1=op1)


def STT(out, in0, scalar, in1, op0, op1):
    return lambda e: e.scalar_tensor_tensor(out=out, in0=in0, scalar=scalar, in1=in1, op0=op0, op1=op1)


def CP(out, in_):
    return lambda e: e.tensor_copy(out=out, in_=in_)


def MS(ap, v):
    return lambda e: e.memset(ap, v)


def RCP(out, in_):
    return lambda e: e.reciprocal(out=out, in_=in_)


def build_program(stop_after=None):
    nc = bass.Bass("TRN2", target_bir_lowering=False)
    dt_in = lambda n, s, d=F32: nc.dram_tensor(n, s, d, kind="ExternalInput").ap()
    x = dt_in("x", [T, D])
    w_ada = dt_in("w_ada", [D, 6 * D])
    b_ada = dt_in("b_ada", [1, 6 * D])
    norm1_g = dt_in("norm1_g", [1, D])
    norm2_g = dt_in("norm2_g", [1, D])
    w_in = dt_in("w_in", [D, 3584])
    w_out = dt_in("w_out", [D, D])
    w_gate = dt_in("w_gate", [D, DFF])
    w_up = dt_in("w_up", [D, DFF])
    w_down = dt_in("w_down", [DFF, D])
    cbd = dt_in("cb", [128, 768], BF16)
    cfd = dt_in("cf", [128, 672])
    kcd = dt_in("kconst", [12, T], BF16)
    qcd = dt_in("qconst", [8, 4, T], BF16)
    out = nc.dram_tensor("out", [T, D], F32, kind="ExternalOutput").ap()
    dbg = None
    if DEBUG:
        dbg = nc.dram_tensor("dbg", [128, 16384], F32, kind="ExternalOutput").ap()

    with contextlib.ExitStack() as st:
        S = Sched(nc, st)
        AW = 51 * 1024 + 512
        arena = st.enter_context(nc.sbuf_tensor("arena", [128, AW], F32))
        pst = [st.enter_context(nc.psum_tensor("psb%d" % i, [128, 512], F32)) for i in range(8)]
        ps = [p[:, :] for p in pst]
        psb = [p[:, :].bitcast(BF16) for p in pst]
        PK = ["ps%d" % i for i in range(8)]

        class Ar:
            off = 0

        def alloc(nbytes):
            o = Ar.off
            Ar.off += (nbytes + 31) // 32 * 8
            assert Ar.off <= AW, ("arena overflow", Ar.off, AW)
            return o

        def V(nbytes, dtype, pat=None, off=None, **kw):
            o = alloc(nbytes) if off is None else off
            ap = arena[:, o:o + nbytes // 4]
            if dtype != F32:
                ap = ap.bitcast(dtype)
            if pat:
                ap = ap.rearrange(pat, **kw)
            return ap

        cb = V(768 * 2, BF16)
        ident = cb[:, 0:128]
        ones_bf = cb[:, 128:256]
        bd_ones = cb[:, 256:384]
        tri128 = cb[:, 384:512]
        tri4 = cb[:, 512:768].rearrange("p (j t) -> p j t", t=64)
        cf = V(672 * 4, F32)
        scanmask = cf[:, 0:512]
        spar = cf[:, 512:544]
        gmask = cf[:, 544:608].rearrange("p (j n) -> p j n", n=8)
        pastmask = cf[:, 608:672].rearrange("p (j n) -> p j n", n=8)
        c_fm = spar[:, 0:8]
        lbl0 = spar[:, 8:12]
        lbl1 = spar[:, 12:16]
        hg_g = spar[:, 16:17]
        gq_raw = spar[:, 17:18]
        gk = spar[:, 18:19]
        small = V(96 * 4, F32)
        epsb = small[:, 0:1]
        cact = small[:, 8:16]
        lb = small[:, 16:20]
        oml = small[:, 20:24]
        ssq = small[:, 24:40]
        rstd = small[:, 40:56]
        sdv = small[:, 56:57]
        gq8 = small[:, 57:58]
        one1 = small[:, 58:59]
        sdv16 = small[:, 64:80]
        modbuf = V(3 * D * 4, F32, "p (s d) -> p s d", d=D)
        gate2 = V(D * 4, F32)
        hT = V(8 * T * 2, BF16, "p (k t) -> p k t", t=T)
        NWB = 4
        wb = [V(8 * 512 * 2, BF16) for _ in range(NWB)]
        WK = ["wb%d" % i for i in range(NWB)]
        wctr = [0]

        def next_wb():
            i = wctr[0] % NWB
            wctr[0] += 1
            return i

        phase_mark = Ar.off

        S.dma("sp", cb, cbd[:, :], writes=["cb"], semkey="cb")
        S.dma("sp", cf, cfd[:, :], writes=["cf"], semkey="cf")
        S.op("dve", MS(epsb, EPS), writes=["eps"])
        S.op("dve", MS(one1, 1.0), writes=["one1"])
        S.op("act", ACTF(cact, c_fm, AF.Exp, scale=-1.0), reads=["cf"], writes=["cact"])
        S.op("dve", TS(cact, cact, 1.0, None, ALU.add), reads=["cact"], writes=["cact"])
        S.op("dve", RCP(cact, cact), reads=["cact"], writes=["cact"])
        S.op("dve", TT(cact, cact, c_fm, ALU.mult), reads=["cact", "cf"], writes=["cact"])
        S.op("dve", TS(gq8, gq_raw, 0.125, None, ALU.mult), reads=["cf"], writes=["gq8"])
        m0 = Ar.off
        cbc = V(8 * 128 * 2, BF16, "p (k m) -> p k m", m=128)
        g1bc = V(D * 4, F32)
        for kc in range(8):
            S.op("act", ACTF(cbc[:, kc, :], ones_bf, AF.Copy, scale=cact[:, kc:kc + 1]),
                 reads=["cb", "cact"], writes=["cbc"])
        S.op("dve", TT(lb, lbl0, lbl1, ALU.subtract), reads=["cf"], writes=["lb"])
        S.op("act", ACTF(lb, lb, AF.Exp, scale=-1.0), reads=["lb"], writes=["lb"])
        S.op("dve", TS(lb, lb, 1.0, None, ALU.add), reads=["lb"], writes=["lb"])
        S.op("dve", RCP(lb, lb), reads=["lb"], writes=["lb"])
        S.op("dve", TS(oml, lb, -1.0, 1.0, ALU.mult, ALU.add), reads=["lb"], writes=["oml"])

        def mod_cols(c0, dsts, keys, bank0):
            for _ in mod_cols_gen(c0, dsts, keys, bank0):
                pass

        def mod_cols_gen(c0, dsts, keys, bank0, cb_ap=None, cb_key="cbc"):
            n = len(dsts)
            for s_ in range(n):
                S.dma("sp", dsts[s_], b_ada[0:1, c0 + s_ * D:c0 + (s_ + 1) * D].partition_broadcast(128)[:, 0, :],
                      writes=[keys[s_]], semkey="modb_" + keys[s_])
            for kc in range(8):
                wi = next_wb()
                bb = 1536 if n == 3 else 1024
                S.dma("pool", wb[wi][:, 0:n * D].rearrange("p (a b) -> p a b", b=bb),
                      w_ada[kc * 128:(kc + 1) * 128, c0:c0 + n * D].rearrange("p (a b) -> p a b", b=bb),
                      writes=[WK[wi]], semkey=WK[wi])
                for j in range(2 * n):
                    S.op("pe", MM(ps[bank0 + j], (cbc if cb_ap is None else cb_ap)[:, kc, :], wb[wi][:, j * 512:(j + 1) * 512],
                                  start=(kc == 0), stop=(kc == 7)),
                         reads=[WK[wi], cb_key], writes=[PK[bank0 + j]], signal=(kc == 7 or j == 2 * n - 1))
                yield
            for j in range(2 * n):
                s_, hh = j // 2, j % 2
                d = dsts[s_][:, hh * 512:(hh + 1) * 512]
                S.op("dve", TT(d, ps[bank0 + j], d, ALU.add), reads=[PK[bank0 + j], keys[s_]], writes=[keys[s_]])
            yield

        def mod_half(half, dsts):
            mod_cols(half * 3072, dsts, ["mod%d_%d" % (half, s_) for s_ in range(3)], 0)

        mod_cols(0, [modbuf[:, 0, :], modbuf[:, 1, :]], ["mod0_0", "mod0_1"], 0)
        S.dma("sp", g1bc, norm1_g[0:1, :].partition_broadcast(128)[:, 0, :], writes=["g1bc"], semkey="g1bc")
        S.op("dve", STT(modbuf[:, 1, :], modbuf[:, 1, :], 1.0, g1bc, ALU.add, ALU.mult),
             reads=["mod0_1", "g1bc"], writes=["mod0_1"])

        xs = [V(D * 4, F32) for _ in range(3)]
        junk = V(D * 2, BF16)
        ntmp = V(D * 4, F32)
        htm = [V(D * 2, BF16) for _ in range(2)]

        xk = lambda tt: ["xs%d" % (tt % 3)]

        def x_tile_stream(tt):
            return xs[tt % 3]

        def norm_pipeline(xin_of, xkeys_of, a_ap, sh_ap, akey, shkey, pre=None, post=None):
            def stats(tt):
                xin = xin_of(tt)
                S.op("act", ACTF(junk, xin, AF.Square, accum_out=ssq[:, tt:tt + 1]), reads=xkeys_of(tt), writes=["junk", "ssq%d" % tt])
                S.op("act", ACTF(sdv16[:, tt:tt + 1], ssq[:, tt:tt + 1], AF.Ln, scale=1.0 / D, bias=epsb),
                     reads=["ssq%d" % tt, "eps"], writes=["sdv%d" % tt])
                S.op("act", ACTF(rstd[:, tt:tt + 1], sdv16[:, tt:tt + 1], AF.Exp, scale=-0.5), reads=["sdv%d" % tt], writes=["rstd%d" % tt])

            def modt(tt):
                xin = xin_of(tt)
                S.op("dve", STT(ntmp, xin, rstd[:, tt:tt + 1], a_ap, ALU.mult, ALU.mult),
                     reads=xkeys_of(tt) + ["rstd%d" % tt, akey], writes=["ntmp"])
                hb = htm[tt % 2]
                S.op("dve", TT(hb, ntmp, sh_ap, ALU.add), reads=["ntmp", shkey], writes=["htm%d" % (tt % 2)])
                pb = 6 + (tt % 2)
                for kc in range(8):
                    S.op("pe", TR(psb[pb][:, kc * 128:(kc + 1) * 128], hb[:, kc * 128:(kc + 1) * 128], ident),
                         reads=["htm%d" % (tt % 2), "cb"], writes=[PK[pb]], signal=(kc == 7))

            def copyt(tt):
                pb = 6 + (tt % 2)
                S.op("act", ACTF(hT[:, :, tt * 128:(tt + 1) * 128], psb[pb].rearrange("p (k t) -> p k t", t=128), AF.Copy),
                     reads=[PK[pb]], writes=[("hT", tt)])

            if pre:
                pre(0)
                if post:
                    post(0)
                pre(1)
                if post:
                    post(1)
            stats(0)
            for tt in range(NT):
                if pre and tt + 2 < NT:
                    pre(tt + 2)
                if tt + 1 < NT:
                    stats(tt + 1)
                modt(tt)
                if post and tt + 2 < NT:
                    post(tt + 2)
                if tt >= 1:
                    copyt(tt - 1)
            copyt(NT - 1)

        norm_pipeline(lambda tt: xs[tt % 3], xk, modbuf[:, 1, :], modbuf[:, 0, :], "mod0_1", "mod0_0",
                      pre=lambda tt: S.dma("sp", xs[tt % 3], x[tt * 128:(tt + 1) * 128, :], writes=["xs%d" % (tt % 3)], semkey="xs%d" % (tt % 3)))
        mod_cols(2048, [modbuf[:, 2, :]], ["mod0_2"], 4)

        def hTq(tq):
            return [("hT", 4 * tq + i) for i in range(4)]

        if stop_after == "norm1":
            dtmp = V(16384 * 4, F32)
            S.op("dve", CP(dtmp, hT.rearrange("p k t -> p (k t)")), reads=[("hT", i) for i in range(NT)], writes=["dtmp"])
            S.dma("sp", dbg[:, :], dtmp, reads=["dtmp"], writes=["dbg"], semkey="dbg")
            S.wait_all("sp", ["dbg"])
            S.emit()
            return nc

        Ar.off = m0
        ALIAS0 = ["cbc", "g1bc", "xs0", "xs1", "xs2", "junk", "ntmp", "htm0", "htm1"]
        mixedT = V(8 * T * 2, BF16, "p (k t) -> p k t", t=T)
        v_hg = V(NT * 512 * 2, BF16, "p (n c) -> p n c", c=512)
        v_att_off = Ar.off
        v_att = V(NT * 768 * 2, BF16, "p (n c) -> p n c", c=768)
        m1 = Ar.off
        first_alias = [True]

        def alias_w():
            if first_alias[0]:
                first_alias[0] = False
                return list(ALIAS0)
            return []

        def load_w_cols(wi, segs):
            wv = wb[wi].rearrange("p (k c) -> p k c", c=512)
            o = 0
            for (c0, n) in segs:
                S.dma("pool", wv[:, :, o:o + n], w_in[:, c0:c0 + n].rearrange("(k p) c -> p k c", p=128),
                      writes=[WK[wi]], semkey=WK[wi])
                o += n
            return wv

        def v_segment_gen(c0, dst, dkey, grouped=False):
            wi = next_wb()
            wv = load_w_cols(wi, [(c0, 512)])
            for tt in range(NT):
                pb = 6 + (tt % 2)
                for kc in range(8):
                    S.op("pe", MM(ps[pb], hT[:, kc, tt * 128:(tt + 1) * 128], wv[:, kc, :], start=(kc == 0), stop=(kc == 7)),
                         reads=[("hT", tt), WK[wi]], writes=[PK[pb]], signal=(kc == 7))
                eng = "act" if tt % 2 == 0 else "dve"
                if grouped:
                    d_ap = dst[:, tt, :].rearrange("p (g a b) -> p g a b", g=4, a=3, b=64)[:, :, 0::2, :]
                    s_ap = ps[pb].rearrange("p (g a b) -> p g a b", g=4, a=2, b=64)
                else:
                    d_ap = dst[:, tt, :]
                    s_ap = ps[pb]
                fn = ACTF(d_ap, s_ap, AF.Copy) if eng == "act" else CP(d_ap, s_ap)
                S.op(eng, fn, reads=[PK[pb]], writes=[(dkey, tt)] + alias_w())
                yield

        def v_segment(c0, dst, dkey, grouped=False):
            for _ in v_segment_gen(c0, dst, dkey, grouped):
                pass

        v_segment(1024, v_hg, "vhg")

        mh = Ar.off
        QB = 512 * 4
        b3 = lambda ap: ap.rearrange("p (c j) -> p c j", j=64)

        def hg_set(n):
            d = {}
            for nm in ("a", "b", "c", "d", "e", "g"):
                d[nm] = V(QB, F32)
            for nm in ("kT", "khT", "qT", "qhT", "gs"):
                d[nm] = V(512 * 2, BF16)
            d["k_tm"] = V(512 * 2, BF16, "p (j k) -> p j k", k=128)
            d["attn"] = V(512 * 2, BF16, "p (j t) -> p j t", t=128)
            d["ab"] = V(16 * 4, F32)
            d["Sb"] = V(8 * 128 * 2, BF16, "p (c v) -> p c v", v=128)
            d["n"] = n
            return d

        HS = [hg_set(0), hg_set(1)]
        Shist = V(9 * 128 * 4, F32, "p (c v) -> p c v", v=128, off=v_att_off)
        osq = V(512 * 2, BF16, off=v_att_off + 1152)
        t_rs = V(QB, F32, off=v_att_off + 1152 + 256)
        t_o = V(QB, F32, off=v_att_off + 1152 + 256 + 512)
        for P in HS:
            S.op("dve", MS(P["attn"], 0.0), writes=["attn%d" % P["n"]])
        hw = {}

        def hg_s1(i):
            h, tq = i // 4, i % 4
            P = HS[i % 2]
            n = P["n"]
            K = lambda s: "%s%d" % (s, n)
            if tq == 0:
                wi = next_wb()
                hw[h] = (wi, load_w_cols(wi, [(h * 128, 128), (512 + h * 128, 128), (1536 + h * 128, 128)]))
            wi, wv = hw[h]
            cols = slice(tq * 512, (tq + 1) * 512)
            a, b, c, d, e = P["a"], P["b"], P["c"], P["d"], P["e"]
            g_ = P["g"]
            for seg in (1, 0, 2):
                for kc in range(8):
                    S.op("pe", MM(ps[seg], wv[:, kc, seg * 128:(seg + 1) * 128], hT[:, kc, cols], start=(kc == 0), stop=(kc == 7)),
                         reads=hTq(tq) + [WK[wi]], writes=[PK[seg]], signal=(kc == 7))
                    if kc % 4 == 3:
                        yield
            S.op("act", ACTF(a, ps[1], AF.Exp, scale=-1.0), reads=[PK[1]], writes=[K("a")])
            S.op("act", ACTF(e, ps[0], AF.Exp, scale=-1.0), reads=[PK[0]], writes=[K("e")])
            S.op("act", ACTF(g_, ps[2], AF.Exp, scale=-1.0), reads=[PK[2]], writes=[K("g")])
            yield
            S.op("act", ACTF(b, a, AF.Ln, bias=one1), reads=[K("a"), "one1"], writes=[K("b")])
            S.op("act", ACTF(e, e, AF.Ln, bias=one1), reads=[K("e"), "one1"], writes=[K("e")])
            S.op("act", ACTF(g_, g_, AF.Ln, bias=one1), reads=[K("g"), "one1"], writes=[K("g")])
            yield
            S.op("act", ACTF(b, b, AF.Exp, scale=-1.0), reads=[K("b")], writes=[K("b")])
            S.op("act", ACTF(e, e, AF.Exp, scale=-1.0), reads=[K("e")], writes=[K("e")])
            S.op("act", ACTF(g_, g_, AF.Exp, scale=-1.0), reads=[K("g")], writes=[K("g")])
            yield
            S.op("dve", STT(a, a, oml[:, h:h + 1], b, ALU.mult, ALU.mult), reads=[K("a"), K("b"), "oml"], writes=[K("a")])
            S.op("dve", TT(e, ps[0], e, ALU.mult), reads=[PK[0], K("e")], writes=[K("e")])
            S.op("dve", TT(P["gs"], ps[2], g_, ALU.mult), reads=[PK[2], K("g")], writes=[K("gs")])
            yield
            S.op("act", ACTF(b, b, AF.Ln, scale=oml[:, h:h + 1], bias=lb[:, h:h + 1]), reads=[K("b"), "oml", "lb"], writes=[K("b")])
            yield
            S.op("dve", lambda e_, b=b, c=c: e_.tensor_tensor_scan(out=c, data0=scanmask, data1=b, initial=0.0, op0=ALU.mult, op1=ALU.add),
                 reads=[K("b"), "cf"], writes=[K("c")])
            S.op("dve", TT(b3(b), b3(c), b3(c)[:, :, 31:32].to_broadcast([128, 8, 64]), ALU.subtract), reads=[K("c")], writes=[K("b")])
            yield
            S.op("act", ACTF(d, b, AF.Exp), reads=[K("b")], writes=[K("d")])
            S.op("act", ACTF(b, b, AF.Exp, scale=-1.0), reads=[K("b")], writes=[K("b")])
            S.op("act", ACTF(P["ab"][:, 0:8], b3(c)[:, :, 63], AF.Exp), reads=[K("c")], writes=[K("ab_a")])
            S.op("act", ACTF(P["ab"][:, 8:16], b3(c)[:, :, 31], AF.Exp), reads=[K("c")], writes=[K("ab_m")])
            yield

        def hg_s2(i):
            h, tq = i // 4, i % 4
            P = HS[i % 2]
            n = P["n"]
            K = lambda s: "%s%d" % (s, n)
            cols = slice(tq * 512, (tq + 1) * 512)
            a, b, c, d, e = P["a"], P["b"], P["c"], P["d"], P["e"]
            S.op("dve", TT(P["kT"], a, b, ALU.mult), reads=[K("a"), K("b")], writes=[K("kT")])
            S.op("dve", TT(b3(P["khT"]), b3(P["kT"]), b3(d)[:, :, 63:64].to_broadcast([128, 8, 64]), ALU.mult),
                 reads=[K("kT"), K("d")], writes=[K("khT")])
            yield
            for j in range(4):
                S.op("pe", TR(psb[7][:, j * 128:(j + 1) * 128], P["khT"][:, j * 128:(j + 1) * 128], ident),
                     reads=[K("khT"), "cb"], writes=[PK[7]], signal=(j == 3))
            S.op("dve", TT(P["qT"], e, d, ALU.mult), reads=[K("e"), K("d")], writes=[K("qT")])
            S.op("dve", TT(b3(P["qhT"]), b3(P["qT"]), P["ab"][:, 8:16].unsqueeze(2).to_broadcast([128, 8, 64]), ALU.mult),
                 reads=[K("qT"), K("ab_m")], writes=[K("qhT")])
            yield
            S.op("act", ACTF(P["k_tm"], psb[7][:, 0:512].rearrange("p (j k) -> p j k", k=128), AF.Copy),
                 reads=[PK[7]], writes=[K("k_tm")])
            for j in range(4):
                S.op("pe", MM(ps[5][:, j * 128:(j + 1) * 128], P["kT"][:, j * 128:(j + 1) * 128], P["qT"][:, j * 128:(j + 1) * 128]),
                     reads=[K("kT"), K("qT")], writes=[PK[5]], signal=(j == 3))
            yield
            for cch in range(8):
                p0 = 64 * (cch % 2)
                S.op("pe", MM(ps[3 + cch % 2][:, (cch // 2) * 128:(cch // 2 + 1) * 128],
                              P["k_tm"][p0:p0 + 64, cch // 2, :], v_hg[p0:p0 + 64, 4 * tq + cch // 2, h * 128:(h + 1) * 128]),
                     reads=[K("k_tm"), ("vhg", 4 * tq + cch // 2)], writes=[PK[3 + cch % 2]], signal=(cch >= 6))
            psA = ps[5].rearrange("p (j t) -> p j t", t=128)
            S.op("dve", STT(P["attn"][0:64, :, 0:64], psA[0:64, :, 0:64], 1.0e30, tri4[0:64], ALU.min, ALU.mult),
                 reads=[PK[5], "cb"], writes=[K("attn")])
            S.op("dve", STT(P["attn"][64:128, :, 64:128], psA[64:128, :, 64:128], 1.0e30, tri4[64:128], ALU.min, ALU.mult),
                 reads=[PK[5], "cb"], writes=[K("attn")])
            yield
            if tq == 0:
                S.op("dve", MS(Shist[:, 0, :], 0.0), writes=[("Sh", 0)])
            else:
                S.op("dve", CP(Shist[:, 0, :], Shist[:, 8, :]), reads=[("Sh", 8)], writes=[("Sh", 0)])
            for cch in range(8):
                S.op("dve", STT(Shist[:, cch + 1, :], Shist[:, cch, :], P["ab"][:, cch:cch + 1],
                                ps[3 + cch % 2][:, (cch // 2) * 128:(cch // 2 + 1) * 128], ALU.mult, ALU.add),
                     reads=[("Sh", cch), K("ab_a"), PK[3 + cch % 2]], writes=[("Sh", cch + 1)])
                if cch % 2 == 1:
                    yield
            S.op("act", ACTF(P["Sb"], Shist[:, 0:8, :], AF.Copy), reads=[("Sh", cch) for cch in range(8)], writes=[K("Sb")])
            yield
            for j in range(4):
                S.op("pe", MM(ps[6][:, j * 128:(j + 1) * 128], v_hg[:, 4 * tq + j, h * 128:(h + 1) * 128], P["attn"][:, j, :],
                              start=True, stop=False),
                     reads=[("vhg", 4 * tq + j), K("attn")], writes=[PK[6]], signal=False)
                for cc in range(2):
                    cch = 2 * j + cc
                    S.op("pe", MM(ps[6][:, j * 128 + cc * 64:j * 128 + cc * 64 + 64], P["Sb"][:, cch, :],
                                  P["qhT"][:, j * 128 + cc * 64:j * 128 + cc * 64 + 64], start=False, stop=(cc == 1)),
                         reads=[K("Sb"), K("qhT")], writes=[PK[6]], signal=(j == 3 and cc == 1))
            yield
            S.op("act", ACTF(osq, ps[6], AF.Square), reads=[PK[6]], writes=["osq"])
            yield
            S.op("pe", MM(ps[7], ones_bf, osq), reads=["osq", "cb"], writes=[PK[7]])
            yield
            S.op("act", ACTF(t_rs, ps[7], AF.Ln, scale=1.0 / 128, bias=epsb), reads=[PK[7], "eps"], writes=["t_rs"])
            yield
            S.op("act", ACTF(t_rs, t_rs, AF.Exp, scale=-0.5), reads=["t_rs"], writes=["t_rs"])
            yield
            S.op("dve", TT(t_o, ps[6], t_rs, ALU.mult), reads=[PK[6], "t_rs"], writes=["t_o"])
            S.op("dve", STT(mixedT[:, h, cols], t_o, hg_g, P["gs"], ALU.mult, ALU.mult), reads=["t_o", K("gs"), "cf"],
                 writes=[("mx", h, tq)])
            yield

        def zip_gens(g1, g2):
            l1 = list_steps = None
            d1 = d2 = False
            while not (d1 and d2):
                if not d2:
                    d2 = next(g2, "done") == "done"
                if not d1:
                    d1 = next(g1, "done") == "done"

        for _ in hg_s1(0):
            pass
        for i in range(16):
            if i + 1 < 16:
                zip_gens(hg_s1(i + 1), hg_s2(i))
            else:
                for _ in hg_s2(i):
                    pass

        if stop_after == "hgrn2":
            S.barrier(engs=("pe", "act", "dve", "sp", "pool"))
            S.dma("pool", dbg[:, 0:8192].rearrange("p (k t) -> p k t", t=T), mixedT[:, 0:4, :], reads=[], writes=["dbg"], semkey="dbg")
            S.wait_all("sp", ["dbg"])
            S.emit()
            return nc

        HG_KEYS = ["osq", "t_rs", "t_o", "Sb0", "Sb1"] + [("Sh", cch) for cch in range(9)]
        for n in range(2):
            HG_KEYS += ["%s%d" % (s, n) for s in ("a", "b", "c", "d", "e", "g", "kT", "khT", "qT", "qhT", "gs", "k_tm", "attn", "ab_a", "ab_m")]
        Ar.off = mh
        S.fence(("act", "dve", "pe"), ["osq", "t_rs", "t_o"] + [("Sh", cch) for cch in range(9)])
        S.op("dve", MS(v_att.rearrange("p n (g a b) -> p (n g) a b", g=4, a=3, b=64)[:, :, 1, :], 1.0), writes=["vones"])
        S.fence(("pe", "act", "dve", "sp"), HG_KEYS)
        vgen = v_segment_gen(3072, v_att, "vatt", grouped=True)
        QA = [[V(T * 2, BF16) for _ in range(2)] for _ in range(2)]
        KA = [[V(T * 2, BF16) for _ in range(2)] for _ in range(2)]
        t_sq = [V(512 * 2, BF16) for _ in range(2)]
        t_r = [V(QB, F32) for _ in range(2)]
        t_rd = V(QB, F32)
        PTb = [V(512 * 2, BF16) for _ in range(4)]
        kms = V(8 * 4, F32)
        kmT = [V(8 * 2, BF16) for _ in range(2)]
        g8 = V(8 * 8 * 4, F32, "p (j n) -> p j n", n=8)
        top8 = V(8 * 8 * 4, F32, "p (j n) -> p j n", n=8)
        stg = V(8 * 72 * 2, BF16, "p (j c) -> p j c", c=72)
        S.op("dve", MS(stg, 0.0), writes=["stg"])
        cbc2 = V(8 * 128 * 2, BF16, "p (k m) -> p k m", m=128)
        for kc in range(8):
            S.op("act", ACTF(cbc2[:, kc, :], ones_bf, AF.Copy, scale=cact[:, kc:kc + 1]), reads=["cb", "cact"], writes=["cbc2"])
        for par in range(2):
            for hh in range(2):
                S.op("dve", MS(QA[par][hh][64:128, :], 0.0), writes=[("qa", par, hh), ("qac", par, hh)])
                S.op("dve", MS(KA[par][hh][64:128, :], 0.0), writes=[("kac", par, hh)])
                S.dma("sp", KA[par][hh][64:76, :], kcd[:, :], writes=[("kac", par, hh)], semkey="kac%d%d" % (par, hh))

        def moba_side(hp):
            par = hp % 2
            wi = next_wb()
            wv = load_w_cols(wi, [(2048 + hp * 128, 128), (2560 + hp * 128, 128)])
            for hh in range(2):
                S.dma("sp", QA[par][hh][72:76, :], qcd[2 * hp + hh, :, :], writes=[("qac", par, hh)], semkey="qac%d%d" % (par, hh))
            for tq in range(4):
                cols = slice(tq * 512, (tq + 1) * 512)
                for seg in range(2):
                    for kc in range(8):
                        S.op("pe", MM(ps[seg], wv[:, kc, seg * 128:(seg + 1) * 128], hT[:, kc, cols], start=(kc == 0), stop=(kc == 7)),
                             reads=hTq(tq) + [WK[wi]], writes=[PK[seg]], signal=(kc == 7))
                        if kc % 2 == 1:
                            yield
                for seg in range(2):
                    S.op("act", ACTF(t_sq[seg], ps[seg], AF.Square), reads=[PK[seg]], writes=["t_sq%d" % seg])
                    yield
                for seg in range(2):
                    S.op("pe", MM(ps[2], bd_ones, t_sq[seg]), reads=["t_sq%d" % seg, "cb"], writes=[PK[2]])
                    yield
                    S.op("act", ACTF(t_r[seg], ps[2], AF.Ln, scale=1.0 / 64, bias=epsb), reads=[PK[2], "eps"], writes=["t_r%d" % seg])
                    yield
                for seg in range(2):
                    S.op("act", ACTF(t_r[seg], t_r[seg], AF.Exp, scale=-0.5), reads=["t_r%d" % seg], writes=["t_r%d" % seg])
                yield
                for seg in range(2):
                    dst = QA[par] if seg == 0 else KA[par]
                    dkey = "qa" if seg == 0 else "ka"
                    gain = gq8 if seg == 0 else gk
                    for hh in range(2):
                        pr = slice(64 * hh, 64 * hh + 64)
                        S.op("dve", STT(dst[hh][0:64, cols], ps[seg][pr, :], gain[pr, :], t_r[seg][pr, :], ALU.mult, ALU.mult),
                             reads=[PK[seg], "t_r%d" % seg, "cf", "gq8"], writes=[(dkey, par, hh)])
                yield
            for hh in range(2):
                qa, ka = QA[par][hh], KA[par][hh]
                S.op("dve", lambda e_, ka=ka: e_.tensor_reduce(out=kms[0:64, :], in_=ka[0:64, :].rearrange("p (n j) -> p n j", j=256),
                                                               axis=AX.X, op=ALU.add),
                     reads=[("ka", par, hh)], writes=["kms"])
                S.op("dve", TS(kmT[hh][0:64, :], kms[0:64, :], 1.0 / 256, None, ALU.mult), reads=["kms"], writes=[("kmT", hh)])
                yield
                for j in range(8):
                    tt = 8 + j
                    S.op("pe", MM(ps[2][:, j * 8:(j + 1) * 8], qa[0:64, tt * 128:(tt + 1) * 128], kmT[hh][0:64, :]),
                         reads=[("qa", par, hh), ("kmT", hh)], writes=[PK[2]], signal=(j == 7))
                yield
                S.op("dve", TT(g8, ps[2][:, 0:64].rearrange("p (j n) -> p j n", n=8), gmask, ALU.add), reads=[PK[2], "cf"], writes=["g8"])
                for j in range(8):
                    S.op("dve", lambda e_, j=j: e_.max(out=top8[:, j, :], in_=g8[:, j, :]), reads=["g8"], writes=["top8"])
                yield
                S.op("dve", TT(g8, g8, top8[:, :, 2:3].to_broadcast([128, 8, 8]), ALU.is_lt), reads=["g8", "top8"], writes=["g8"])
                S.op("dve", TT(stg[:, :, 64:72], g8, pastmask, ALU.mult), reads=["g8", "cf"], writes=["stg"])
                yield
                for j in range(8):
                    S.op("pe", TR(psb[2][0:72, j * 128:(j + 1) * 128], stg[:, j, :], ident), reads=["stg", "cb"], writes=[PK[2]],
                         signal=(j == 7))
                yield
                S.op("act", ACTF(qa[64:72, 1024:2048], psb[2][64:72, 0:1024], AF.Copy), reads=[PK[2]], writes=[("qa", par, hh)])
                yield

        SCB = [4, 5, 7]
        ITS = []
        for hp_ in range(4):
            for hh_ in range(2):
                for tq_ in range(4):
                    for j_ in range(4 * tq_ + 4):
                        ITS.append((hp_, hh_, tq_, j_, 4 * tq_ + 4))
        side_gen = {}
        side_done = set()

        def ensure_pair(hp):
            if hp in side_done:
                return
            g_ = side_gen.get(hp)
            if g_ is None:
                g_ = moba_side(hp)
            for _ in g_:
                pass
            side_done.add(hp)

        def qk(g):
            hp, hh, tq, j, nkt = ITS[g]
            ensure_pair(hp)
            par = hp % 2
            qa, ka = QA[par][hh], KA[par][hh]
            t0 = max(j, 4 * tq)
            ncol = (4 * tq + 4 - t0) * 128
            pbk = SCB[g % 3]
            S.op("pe", MM(ps[pbk][:, 0:ncol], ka[:, j * 128:(j + 1) * 128], qa[:, t0 * 128:t0 * 128 + ncol]),
                 reads=[("ka", par, hh), ("kac", par, hh), ("qa", par, hh), ("qac", par, hh)], writes=[PK[pbk]])

        side0 = moba_side(0)
        d1 = d2 = False
        while not (d1 and d2):
            if not d1:
                d1 = next(vgen, "done") == "done"
            for _ in range(4):
                if not d2:
                    d2 = next(side0, "done") == "done"
        side_done.add(0)

        qk(0)
        qk(1)
        pending = []
        qcount = 0
        for g in range(len(ITS)):
            hp, hh, tq, j, nkt = ITS[g]
            par = hp % 2
            h = 2 * hp + hh
            if hh == 0 and tq == 0 and j == 0 and hp + 1 < 4:
                side_gen[hp + 1] = moba_side(hp + 1)
            if hh == 0 and tq == 0 and j == 0 and hp == 3:
                def mod2_side():
                    for ci_, (dst_, key_) in enumerate(((modbuf[:, 0, :], "mod1_0"), (modbuf[:, 1, :], "mod1_1"), (gate2, "mod1_2"))):
                        for _ in mod_cols_gen(3072 + ci_ * D, [dst_], [key_], 0, cb_ap=cbc2, cb_key="cbc2"):
                            yield
                side_gen[4] = mod2_side()
            nump = slice(64 * hh, 64 * hh + 64)
            denp = slice(64 * (1 - hh), 64 * (1 - hh) + 64)
            vcol = hp * 192 + hh * 64
            t0 = max(j, 4 * tq)
            ncol = (4 * tq + 4 - t0) * 128
            lo = (t0 - 4 * tq) * 128
            pbk = SCB[g % 3]
            pt = PTb[g % 4]
            ptk = "PT%d" % (g % 4)
            bn = 6 if qcount % 2 == 0 else 3
            if g + 2 < len(ITS):
                qk(g + 2)
            S.op("act", ACTF(pt[:, 0:ncol], ps[pbk][:, 0:ncol], AF.Exp), reads=[PK[pbk]], writes=[ptk])
            if j >= 4 * tq:
                S.op("dve", TT(pt[:, 0:128], pt[:, 0:128], tri128, ALU.mult), reads=[ptk, "cb"], writes=[ptk])
            S.op("pe", MM(ps[bn][:, lo:lo + ncol], v_att[:, j, vcol:vcol + 128], pt[:, 0:ncol],
                          start=(j == 0), stop=(j == nkt - 1)),
                 reads=[("vatt", j), "vones", ptk], writes=[PK[bn]], signal=True)
            if j == nkt - 1:
                def norm(bn=bn, nump=nump, denp=denp, hp=hp, tq=tq, hh=hh):
                    S.op("act", ACTF(t_rd[nump, :], ps[bn][denp, :], AF.Ln), reads=[PK[bn]], writes=["t_rd"])
                    S.op("act", ACTF(t_rd[nump, :], t_rd[nump, :], AF.Exp, scale=-1.0), reads=["t_rd"], writes=["t_rd"])
                    S.op("dve", TT(mixedT[nump, 4 + hp, tq * 512:(tq + 1) * 512], ps[bn][nump, :], t_rd[nump, :], ALU.mult),
                         reads=[PK[bn], "t_rd"], writes=[("mx", 4 + hp, tq, hh)])
                pending.append((g + 3, norm))
                qcount += 1
            while pending and pending[0][0] <= g:
                pending.pop(0)[1]()
            sg = side_gen.get(hp + 1)
            if sg is not None and (hp + 1) not in side_done:
                if next(sg, "done") == "done":
                    side_done.add(hp + 1)
        for _, fn in pending:
            fn()
        if 4 in side_gen and 4 not in side_done:
            for _ in side_gen[4]:
                pass
            side_done.add(4)

        if stop_after == "moba":
            S.barrier(engs=("pe", "act", "dve", "sp", "pool"))
            S.dma("pool", dbg[:, 0:8192].rearrange("p (k t) -> p k t", t=T), mixedT[:, 4:8, :], reads=[], writes=["dbg"], semkey="dbg")
            S.wait_all("sp", ["dbg"])
            S.emit()
            return nc

        wis = []
        for hf in range(2):
            wi = next_wb()
            wv = wb[wi].rearrange("p (k c) -> p k c", c=512)
            S.dma("pool", wv, w_out[:, hf * 512:(hf + 1) * 512].rearrange("(k p) c -> p k c", p=128), writes=[WK[wi]], semkey=WK[wi])
            S.op("dve", TT(wv, wv, modbuf[:, 2, hf * 512:(hf + 1) * 512].unsqueeze(1).to_broadcast([128, 8, 512]), ALU.mult),
                 reads=[WK[wi], "mod0_2"], writes=[WK[wi]])
            wis.append((wi, wv))
        S.barrier()
        Ar.off = m0 + 8 * T * 2 // 4
        x1 = V(NT * D * 4, F32, "p (n d) -> p n d", d=D)
        m2 = Ar.off
        cbc_off = Ar.off
        cbc = V(8 * 128 * 2, BF16, "p (k m) -> p k m", m=128)
        g2bc_off = Ar.off
        g2bc = V(D * 4, F32)
        otmp_off = Ar.off
        otmp = [V(512 * 4, F32) for _ in range(2)]
        S.dma("sp", g2bc, norm2_g[0:1, :].partition_broadcast(128)[:, 0, :], writes=["g2bc"], semkey="g2bc")
        for tt in range(NT):
            S.dma("sp", x1[:, tt, :], x[tt * 128:(tt + 1) * 128, :], writes=[("x1", tt)], semkey="x1_%d" % tt)
        S.op("dve", STT(modbuf[:, 1, :], modbuf[:, 1, :], 1.0, g2bc, ALU.add, ALU.mult), reads=["mod1_1", "g2bc"], writes=["mod1_1"])
        junk = V(D * 2, BF16)
        ntmp = V(D * 4, F32, off=otmp_off)
        htm = [V(D * 2, BF16) for _ in range(2)]
        opit = [0]

        OPB = [2, 3, 4, 5]

        def outproj_pe(tt):
            for hf in range(2):
                wi, wv = wis[hf]
                pb = OPB[(2 * tt + hf) % 4]
                for kc in range(8):
                    S.op("pe", MM(ps[pb], mixedT[:, kc, tt * 128:(tt + 1) * 128], wv[:, kc, :], start=(kc == 0), stop=(kc == 7)),
                         reads=[WK[wi]], writes=[PK[pb]], signal=(kc == 7))

        def outproj_dve(tt):
            for hf in range(2):
                pb = OPB[(2 * tt + hf) % 4]
                S.op("dve", TT(x1[:, tt, hf * 512:(hf + 1) * 512], ps[pb], x1[:, tt, hf * 512:(hf + 1) * 512], ALU.add),
                     reads=[PK[pb], ("x1", tt)], writes=[("x1", tt)])

        norm_pipeline(lambda tt: x1[:, tt, :], lambda tt: [("x1", tt)], modbuf[:, 1, :], modbuf[:, 0, :], "mod1_1", "mod1_0",
                      pre=outproj_pe, post=outproj_dve)

        if stop_after == "x1":
            for tt in range(NT):
                S.dma("sp", out[tt * 128:(tt + 1) * 128, :], x1[:, tt, :], reads=[("x1", tt)], writes=["out"], semkey="out")
            S.wait_all("sp", ["out"])
            S.emit()
            return nc

        S.barrier(engs=("pe", "act", "dve"))
        Ar.off = m0
        aT = [V(4 * T * 2, BF16, "p (c t) -> p c t", t=T) for _ in range(2)]
        sgb = [V(512 * 2, BF16, off=cbc_off + i * 256) for i in range(2)]
        assert Ar.off <= m0 + 8 * T * 2 // 4
        assert otmp_off == g2bc_off + 1024
        wb.append(V(8 * 512 * 2, BF16, off=g2bc_off))
        wb.append(modbuf[:, 0:2, :].rearrange("p s d -> p (s d)").bitcast(BF16))
        WK.extend(["wb4", "wb5"])
        EXTRA_ALIAS = {4: ["g2bc", "ntmp", "otmp0", "otmp1"], 5: ["mod1_0", "mod1_1"]}
        fctr = [0]
        first_use = set()

        def next_fwb():
            i = fctr[0] % 6
            fctr[0] += 1
            extra = []
            if i in EXTRA_ALIAS and i not in first_use:
                first_use.add(i)
                extra = EXTRA_ALIAS[i]
            return i, extra

        groups = [(g * 4, 4) for g in range(5)] + [(20, 2)]
        for gi, (c0, nch) in enumerate(groups):
            a = aT[gi % 2]
            ak = "aT%d" % (gi % 2)
            wg_i, xg = next_fwb()
            wu_i, xu = next_fwb()
            wgv = wb[wg_i].rearrange("p (k c) -> p k c", c=512)
            wuv = wb[wu_i].rearrange("p (k c) -> p k c", c=512)
            ncol = nch * 128
            S.dma("pool", wgv[:, :, 0:ncol], w_gate[:, c0 * 128:c0 * 128 + ncol].rearrange("(k p) c -> p k c", p=128),
                  writes=[WK[wg_i]] + xg, semkey=WK[wg_i])
            S.dma("pool", wuv[:, :, 0:ncol], w_up[:, c0 * 128:c0 * 128 + ncol].rearrange("(k p) c -> p k c", p=128),
                  writes=[WK[wu_i]] + xu, semkey=WK[wu_i])
            it = 0
            for ci in range(nch):
                for tq in range(4):
                    cols = slice(tq * 512, (tq + 1) * 512)
                    pg, pu = (0, 1) if it % 2 == 0 else (2, 3)
                    sg = sgb[it % 2]
                    sgk = "sg%d" % (it % 2)
                    it += 1
                    for kc in range(8):
                        S.op("pe", MM(ps[pg], wgv[:, kc, ci * 128:(ci + 1) * 128], hT[:, kc, cols], start=(kc == 0), stop=(kc == 7)),
                             reads=hTq(tq) + [WK[wg_i]], writes=[PK[pg]], signal=(kc == 7))
                    for kc in range(8):
                        S.op("pe", MM(ps[pu], wuv[:, kc, ci * 128:(ci + 1) * 128], hT[:, kc, cols], start=(kc == 0), stop=(kc == 7)),
                             reads=hTq(tq) + [WK[wu_i]], writes=[PK[pu]], signal=(kc == 7))
                    S.op("act", ACTF(sg, ps[pg], AF.Silu), reads=[PK[pg]], writes=[sgk])
                    S.op("dve", TT(a[:, ci, cols], sg, ps[pu], ALU.mult), reads=[sgk, PK[pu]], writes=[(ak, ci, tq)])
            wd_i, xd = next_fwb()
            wdv = wb[wd_i].rearrange("p (j c) -> p j c", c=1024)
            S.dma("pool", wdv[:, 0:nch, :], w_down[c0 * 128:(c0 + nch) * 128, :].rearrange("(j p) c -> p j c", p=128),
                  writes=[WK[wd_i]] + xd, semkey=WK[wd_i])
            S.op("dve", TT(wdv[:, 0:nch, :], wdv[:, 0:nch, :], gate2.unsqueeze(1).to_broadcast([128, nch, D]), ALU.mult),
                 reads=[WK[wd_i], "mod1_2"], writes=[WK[wd_i]])
            it = 0
            for tt in range(NT):
                for hf in range(2):
                    pb = 4 + (it % 3)
                    ot = otmp[it % 2]
                    otk = "otmp%d" % (it % 2)
                    it += 1
                    for ci in range(nch):
                        S.op("pe", MM(ps[pb], a[:, ci, tt * 128:(tt + 1) * 128], wdv[:, ci, hf * 512:(hf + 1) * 512],
                                      start=(ci == 0), stop=(ci == nch - 1)),
                             reads=[(ak, ci, tt // 4), WK[wd_i]], writes=[PK[pb]], signal=(ci == nch - 1))
                    S.op("dve", TT(x1[:, tt, hf * 512:(hf + 1) * 512], ps[pb], x1[:, tt, hf * 512:(hf + 1) * 512], ALU.add),
                         reads=[PK[pb], ("x1", tt)], writes=[("x1", tt)])
                if gi == len(groups) - 1:
                    S.dma("sp", out[tt * 128:(tt + 1) * 128, :], x1[:, tt, :], reads=[("x1", tt)], writes=["out"], semkey="out")
        S.wait_all("sp", ["out"])
        S.emit()
    return nc


def _consts():
    bf = ml_dtypes.bfloat16
    cb = np.zeros((128, 768), np.float32)
    p = np.arange(128)
    cb[:, 0:128] = np.eye(128)
    cb[:, 128:256] = 1.0
    cb[:, 256:384] = (p[:, None] // 64 == p[None, :] // 64)
    cb[:, 384:512] = (p[:, None] <= p[None, :])
    t64 = np.arange(64)
    tri4 = ((p[:, None] % 64) <= t64[None, :]).astype(np.float32)
    cb[:, 512:768] = np.tile(tri4[:, None, :], (1, 4, 1)).reshape(128, 256)
    scan = np.ones((128, 512), np.float32)
    scan[:, 0::64] = 0.0
    t = np.arange(T)
    hi16 = (t // 16) * 16.0
    lo = (t % 16).astype(np.float32)
    kc = np.zeros((12, T), np.float32)
    for n in range(8):
        kc[n] = -BIG * (t // 256 == n)
    kc[8] = 1.0
    kc[9] = 1.0
    kc[10] = hi16
    kc[11] = lo
    qc = np.zeros((8, 4, T), np.float32)
    for h in range(8):
        sl = 2.0 ** (-(h + 1))
        qc[h, 0] = -sl * hi16
        qc[h, 1] = -sl * lo
        qc[h, 2] = sl
        qc[h, 3] = sl
    return cb.astype(bf), scan, kc.astype(bf), qc.astype(bf)


_CACHE = {}


def kernel(x, c, w_ada, b_ada, norm1_g, w_in, lb_logits, hg_norm_g, q_norm_g, k_norm_g,
           w_out, norm2_g, w_gate, w_up, w_down):
    f = lambda a: np.ascontiguousarray(np.asarray(a, dtype=np.float32))
    x = f(x); c = f(c)
    stop_after = DEBUG or None
    key = ("nc", stop_after)
    if key not in _CACHE:
        _CACHE[key] = build_program(stop_after)
    nc = _CACHE[key]
    cb, scan, kc, qc = _consts()
    lbl = f(lb_logits)
    shared = {
        "w_ada": f(w_ada)[0], "b_ada": f(b_ada), "norm1_g": f(norm1_g), "norm2_g": f(norm2_g),
        "w_in": f(w_in)[0], "w_out": f(w_out)[0], "w_gate": f(w_gate)[0], "w_up": f(w_up)[0], "w_down": f(w_down)[0],
        "cb": cb, "kconst": kc, "qconst": qc,
    }
    in_maps = []
    for b in range(8):
        cf = np.zeros((128, 672), np.float32)
        cf[:, 0:512] = scan
        for j in range(8):
            qb = (8 + j) // 2
            cf[:, 544 + j * 8 + qb:544 + (j + 1) * 8] = NEG
            cf[:, 608 + j * 8:608 + j * 8 + qb] = 1.0
        cf[:, 512:520] = c[b].reshape(8, 128).T
        cf[:, 520:524] = lbl[0].reshape(4, 128).T
        cf[:, 524:528] = lbl[1].reshape(4, 128).T
        cf[:, 528] = f(hg_norm_g)[0]
        cf[:, 529] = np.tile(f(q_norm_g)[0], 2)
        cf[:, 530] = np.tile(f(k_norm_g)[0], 2)
        m = dict(shared)
        m["x"] = x[b]
        m["cf"] = cf
        in_maps.append(m)
    res = run_bass_kernel_spmd(nc, in_maps, core_ids=list(range(8)))
    if DEBUG and DEBUG != "x1":
        return np.stack([r["dbg"] for r in res.results], axis=0)
    return np.stack([r["out"] for r in res.results], axis=0).astype(np.float32)
```

```python
import contextlib
import os
import numpy as np
import ml_dtypes
import concourse.bass as bass
import concourse.mybir as mybir
from concourse.bass_utils import run_bass_kernel_spmd

F32 = mybir.dt.float32
BF16 = mybir.dt.bfloat16
ALU = mybir.AluOpType
AF = mybir.ActivationFunctionType
AX = mybir.AxisListType

T = 2048
D = 1024
NT = 16
DFF = 2816
NFF = 22
EPS = 1e-6
BIG = 30000.0
NEG = -1.0e30
ENGS = ("pe", "act", "dve", "pool", "sp")
DEBUG = os.environ.get("KDEBUG", "")


class Sched:
    def __init__(self, nc, stack):
        self.nc = nc
        self.ops = {e: [] for e in ENGS}
        self.cnt = {e: 0 for e in ENGS}
        self.nops = {e: 0 for e in ENGS}
        self.waited = {e: {} for e in ENGS}
        self.res = {}
        self.esem = {e: stack.enter_context(nc.semaphore("c_" + e)) for e in ENGS if e != "sp"}
        self.dsem = {}
        self.dcnt = {}
        self.stack = stack
        self.pending = {e: [] for e in ENGS}

    def _dma_sem(self, key):
        if key not in self.dsem:
            self.dsem[key] = self.stack.enter_context(self.nc.semaphore("d_%d" % len(self.dsem)))
            self.dcnt[key] = 0
        return self.dsem[key]

    def _add(self, eng, waits, sem, val):
        if self.waited[eng].get(id(sem), 0) >= val:
            return
        old = waits.get(id(sem))
        if old is None or old[1] < val:
            waits[id(sem)] = (sem, val)

    def _need(self, eng, waits, tok):
        if tok is None:
            return
        sem, val, peng, pidx = tok
        if peng == eng:
            return
        self._add(eng, waits, sem, val)

    def _deps(self, eng, reads, writes):
        waits = {}
        my_idx = self.nops[eng]
        for k in reads:
            r = self.res.get(k)
            if r and r[0] is not None:
                tok = r[0]
                self._need(eng, waits, tok)
                if tok[2] == eng and eng in ("act", "dve", "pool"):
                    self._add(eng, waits, tok[0], tok[1])
        for k in writes:
            r = self.res.get(k)
            if r:
                self._need(eng, waits, r[0])
                for tok in r[1]:
                    self._need(eng, waits, tok)
                if eng in ("act", "dve", "pool"):
                    for tok in ([r[0]] if r[0] is not None else []) + r[1]:
                        if tok[2] == eng:
                            self._add(eng, waits, tok[0], tok[1])
        out = []
        for sid, (sem, val) in waits.items():
            self.waited[eng][sid] = val
            out.append((sem, val))
        return out

    def _commit(self, reads, writes, tok):
        for k in reads:
            r = self.res.setdefault(k, [None, []])
            r[1].append(tok)
        for k in writes:
            self.res[k] = [tok, []]

    def op(self, eng, fn, reads=(), writes=(), signal=True):
        waits = self._deps(eng, reads, writes)
        idx = self.nops[eng]
        self.nops[eng] += 1
        if signal:
            self.cnt[eng] += 1
            tok = (self.esem[eng], self.cnt[eng], eng, idx)
            self.ops[eng].append((waits, fn, (self.esem[eng], 1)))
            for (rr, ww) in self.pending[eng]:
                self._commit(rr, ww, tok)
            self.pending[eng] = []
            self._commit(reads, writes, tok)
        else:
            self.ops[eng].append((waits, fn, None))
            self.pending[eng].append((list(reads), list(writes)))

    def dma(self, eng, out, in_, reads=(), writes=(), semkey=None):
        waits = self._deps(eng, reads, writes)
        sem = self._dma_sem(semkey)
        self.dcnt[semkey] += 16
        tok = (sem, self.dcnt[semkey], "dma", -1)
        self.nops[eng] += 1
        self.ops[eng].append((waits, (lambda e: e.dma_start(out=out, in_=in_)), (sem, 16)))
        self._commit(reads, writes, tok)

    def barrier(self, engs=("pe", "act", "dve", "sp")):
        for e in engs:
            waits = {}
            for o in ("pe", "act", "dve"):
                if o != e and self.cnt[o] > 0:
                    self._add(e, waits, self.esem[o], self.cnt[o])
            lst = []
            for sid, (sem, val) in waits.items():
                self.waited[e][sid] = val
                lst.append((sem, val))
            self.ops[e].append((lst, None, None))

    def fence(self, engs, keys):
        for e in engs:
            waits = self._deps(e, (), keys)
            self.ops[e].append((waits, None, None))

    def wait_all(self, eng, keys):
        waits = self._deps(eng, keys, ())
        self.ops[eng].append((waits, None, None))

    def emit(self):
        nc = self.nc
        with nc.Block() as block:
            def mk(ename):
                def body(e):
                    for waits, fn, inc in self.ops[ename]:
                        for sem, val in waits:
                            e.wait_ge(sem, val)
                        if fn is None:
                            continue
                        ins = fn(e)
                        if inc is not None:
                            ins.then_inc(inc[0], inc[1])
                return body
            block.tensor(mk("pe"))
            block.scalar(mk("act"))
            block.vector(mk("dve"))
            block.gpsimd(mk("pool"))
            block.sync(mk("sp"))


def MM(out, lhsT, rhs, start=True, stop=True):
    return lambda e: e.matmul(out, lhsT=lhsT, rhs=rhs, start=start, stop=stop)


def TR(out, in_, ident):
    return lambda e: e.transpose(out, in_, ident)


def ACTF(out, in_, func, **kw):
    return lambda e: e.activation(out=out, in_=in_, func=func, **kw)


def TT(out, in0, in1, op):
    return lambda e: e.tensor_tensor(out=out, in0=in0, in1=in1, op=op)


def TS(out, in0, s1, s2, op0, op1=None):
    if op1 is None:
        return lambda e: e.tensor_scalar(out=out, in0=in0, scalar1=s1, scalar2=None, op0=op0)
    return lambda e: e.tensor_scalar(out=out, in0=in0, scalar1=s1, scalar2=s2, op0=op0, op1=op1)


def STT(out, in0, scalar, in1, op0, op1):
    return lambda e: e.scalar_tensor_tensor(out=out, in0=in0, scalar=scalar, in1=in1, op0=op0, op1=op1)


def CP(out, in_):
    return lambda e: e.tensor_copy(out=out, in_=in_)


def MS(ap, v):
    return lambda e: e.memset(ap, v)


def RCP(out, in_):
    return lambda e: e.reciprocal(out=out, in_=in_)


def build_program(stop_after=None):
    nc = bass.Bass("TRN2", target_bir_lowering=False)
    dt_in = lambda n, s, d=F32: nc.dram_tensor(n, s, d, kind="ExternalInput").ap()
    x = dt_in("x", [T, D])
    w_ada = dt_in("w_ada", [D, 6 * D])
    b_ada = dt_in("b_ada", [1, 6 * D])
    norm1_g = dt_in("norm1_g", [1, D])
    norm2_g = dt_in("norm2_g", [1, D])
    w_in = dt_in("w_in", [D, 3584])
    w_out = dt_in("w_out", [D, D])
    w_gate = dt_in("w_gate", [D, DFF])
    w_up = dt_in("w_up", [D, DFF])
    w_down = dt_in("w_down", [DFF, D])
    cbd = dt_in("cb", [128, 768], BF16)
    cfd = dt_in("cf", [128, 672])
    kcd = dt_in("kconst", [12, T], BF16)
    qcd = dt_in("qconst", [8, 4, T], BF16)
    out = nc.dram_tensor("out", [T, D], F32, kind="ExternalOutput").ap()
    dbg = None
    if DEBUG:
        dbg = nc.dram_tensor("dbg", [128, 16384], F32, kind="ExternalOutput").ap()

    with contextlib.ExitStack() as st:
        S = Sched(nc, st)
        AW = 51 * 1024 + 512
        arena = st.enter_context(nc.sbuf_tensor("arena", [128, AW], F32))
        pst = [st.enter_context(nc.psum_tensor("psb%d" % i, [128, 512], F32)) for i in range(8)]
        ps = [p[:, :] for p in pst]
        psb = [p[:, :].bitcast(BF16) for p in pst]
        PK = ["ps%d" % i for i in range(8)]

        class Ar:
            off = 0

        def alloc(nbytes):
            o = Ar.off
            Ar.off += (nbytes + 31) // 32 * 8
            assert Ar.off <= AW, ("arena overflow", Ar.off, AW)
            return o

        def V(nbytes, dtype, pat=None, off=None, **kw):
            o = alloc(nbytes) if off is None else off
            ap = arena[:, o:o + nbytes // 4]
            if dtype != F32:
                ap = ap.bitcast(dtype)
            if pat:
                ap = ap.rearrange(pat, **kw)
            return ap

        cb = V(768 * 2, BF16)
        ident = cb[:, 0:128]
        ones_bf = cb[:, 128:256]
        bd_ones = cb[:, 256:384]
        tri128 = cb[:, 384:512]
        tri4 = cb[:, 512:768].rearrange("p (j t) -> p j t", t=64)
        cf = V(672 * 4, F32)
        scanmask = cf[:, 0:512]
        spar = cf[:, 512:544]
        gmask = cf[:, 544:608].rearrange("p (j n) -> p j n", n=8)
        pastmask = cf[:, 608:672].rearrange("p (j n) -> p j n", n=8)
        c_fm = spar[:, 0:8]
        lbl0 = spar[:, 8:12]
        lbl1 = spar[:, 12:16]
        hg_g = spar[:, 16:17]
        gq_raw = spar[:, 17:18]
        gk = spar[:, 18:19]
        small = V(96 * 4, F32)
        epsb = small[:, 0:1]
        cact = small[:, 8:16]
        lb = small[:, 16:20]
        oml = small[:, 20:24]
        ssq = small[:, 24:40]
        rstd = small[:, 40:56]
        sdv = small[:, 56:57]
        gq8 = small[:, 57:58]
        one1 = small[:, 58:59]
        sdv16 = small[:, 64:80]
        modbuf = V(3 * D * 4, F32, "p (s d) -> p s d", d=D)
        gate2 = V(D * 4, F32)
        hT = V(8 * T * 2, BF16, "p (k t) -> p k t", t=T)
        NWB = 4
        wb = [V(8 * 512 * 2, BF16) for _ in range(NWB)]
        WK = ["wb%d" % i for i in range(NWB)]
        wctr = [0]

        def next_wb():
            i = wctr[0] % NWB
            wctr[0] += 1
            return i

        phase_mark = Ar.off

        S.dma("sp", cb, cbd[:, :], writes=["cb"], semkey="cb")
        S.dma("sp", cf, cfd[:, :], writes=["cf"], semkey="cf")
        S.op("dve", MS(epsb, EPS), writes=["eps"])
        S.op("dve", MS(one1, 1.0), writes=["one1"])
        S.op("act", ACTF(cact, c_fm, AF.Exp, scale=-1.0), reads=["cf"], writes=["cact"])
        S.op("dve", TS(cact, cact, 1.0, None, ALU.add), reads=["cact"], writes=["cact"])
        S.op("dve", RCP(cact, cact), reads=["cact"], writes=["cact"])
        S.op("dve", TT(cact, cact, c_fm, ALU.mult), reads=["cact", "cf"], writes=["cact"])
        S.op("dve", TS(gq8, gq_raw, 0.125, None, ALU.mult), reads=["cf"], writes=["gq8"])
        m0 = Ar.off
        cbc = V(8 * 128 * 2, BF16, "p (k m) -> p k m", m=128)
        g1bc = V(D * 4, F32)
        for kc in range(8):
            S.op("act", ACTF(cbc[:, kc, :], ones_bf, AF.Copy, scale=cact[:, kc:kc + 1]),
                 reads=["cb", "cact"], writes=["cbc"])
        S.op("dve", TT(lb, lbl0, lbl1, ALU.subtract), reads=["cf"], writes=["lb"])
        S.op("act", ACTF(lb, lb, AF.Exp, scale=-1.0), reads=["lb"], writes=["lb"])
        S.op("dve", TS(lb, lb, 1.0, None, ALU.add), reads=["lb"], writes=["lb"])
        S.op("dve", RCP(lb, lb), reads=["lb"], writes=["lb"])
        S.op("dve", TS(oml, lb, -1.0, 1.0, ALU.mult, ALU.add), reads=["lb"], writes=["oml"])

        def mod_cols(c0, dsts, keys, bank0):
            for _ in mod_cols_gen(c0, dsts, keys, bank0):
                pass

        def mod_cols_gen(c0, dsts, keys, bank0, cb_ap=None, cb_key="cbc"):
            n = len(dsts)
            for s_ in range(n):
                S.dma("sp", dsts[s_], b_ada[0:1, c0 + s_ * D:c0 + (s_ + 1) * D].partition_broadcast(128)[:, 0, :],
                      writes=[keys[s_]], semkey="modb_" + keys[s_])
            for kc in range(8):
                wi = next_wb()
                bb = 1536 if n == 3 else 1024
                S.dma("pool", wb[wi][:, 0:n * D].rearrange("p (a b) -> p a b", b=bb),
                      w_ada[kc * 128:(kc + 1) * 128, c0:c0 + n * D].rearrange("p (a b) -> p a b", b=bb),
                      writes=[WK[wi]], semkey=WK[wi])
                for j in range(2 * n):
                    S.op("pe", MM(ps[bank0 + j], (cbc if cb_ap is None else cb_ap)[:, kc, :], wb[wi][:, j * 512:(j + 1) * 512],
                                  start=(kc == 0), stop=(kc == 7)),
                         reads=[WK[wi], cb_key], writes=[PK[bank0 + j]], signal=(kc == 7 or j == 2 * n - 1))
                yield
            for j in range(2 * n):
                s_, hh = j // 2, j % 2
                d = dsts[s_][:, hh * 512:(hh + 1) * 512]
                S.op("dve", TT(d, ps[bank0 + j], d, ALU.add), reads=[PK[bank0 + j], keys[s_]], writes=[keys[s_]])
            yield

        def mod_half(half, dsts):
            mod_cols(half * 3072, dsts, ["mod%d_%d" % (half, s_) for s_ in range(3)], 0)

        mod_cols(0, [modbuf[:, 0, :], modbuf[:, 1, :]], ["mod0_0", "mod0_1"], 0)
        S.dma("sp", g1bc, norm1_g[0:1, :].partition_broadcast(128)[:, 0, :], writes=["g1bc"], semkey="g1bc")
        S.op("dve", STT(modbuf[:, 1, :], modbuf[:, 1, :], 1.0, g1bc, ALU.add, ALU.mult),
             reads=["mod0_1", "g1bc"], writes=["mod0_1"])

        xs = [V(D * 4, F32) for _ in range(3)]
        junk = V(D * 2, BF16)
        ntmp = V(D * 4, F32)
        htm = [V(D * 2, BF16) for _ in range(2)]

        xk = lambda tt: ["xs%d" % (tt % 3)]

        def x_tile_stream(tt):
            return xs[tt % 3]

        def norm_pipeline(xin_of, xkeys_of, a_ap, sh_ap, akey, shkey, pre=None, post=None):
            def stats(tt):
                xin = xin_of(tt)
                S.op("act", ACTF(junk, xin, AF.Square, accum_out=ssq[:, tt:tt + 1]), reads=xkeys_of(tt), writes=["junk", "ssq%d" % tt])
                S.op("act", ACTF(sdv16[:, tt:tt + 1], ssq[:, tt:tt + 1], AF.Ln, scale=1.0 / D, bias=epsb),
                     reads=["ssq%d" % tt, "eps"], writes=["sdv%d" % tt])
                S.op("act", ACTF(rstd[:, tt:tt + 1], sdv16[:, tt:tt + 1], AF.Exp, scale=-0.5), reads=["sdv%d" % tt], writes=["rstd%d" % tt])

            def modt(tt):
                xin = xin_of(tt)
                S.op("dve", STT(ntmp, xin, rstd[:, tt:tt + 1], a_ap, ALU.mult, ALU.mult),
                     reads=xkeys_of(tt) + ["rstd%d" % tt, akey], writes=["ntmp"])
                hb = htm[tt % 2]
                S.op("dve", TT(hb, ntmp, sh_ap, ALU.add), reads=["ntmp", shkey], writes=["htm%d" % (tt % 2)])
                pb = 6 + (tt % 2)
                for kc in range(8):
                    S.op("pe", TR(psb[pb][:, kc * 128:(kc + 1) * 128], hb[:, kc * 128:(kc + 1) * 128], ident),
                         reads=["htm%d" % (tt % 2), "cb"], writes=[PK[pb]], signal=(kc == 7))

            def copyt(tt):
                pb = 6 + (tt % 2)
                S.op("act", ACTF(hT[:, :, tt * 128:(tt + 1) * 128], psb[pb].rearrange("p (k t) -> p k t", t=128), AF.Copy),
                     reads=[PK[pb]], writes=[("hT", tt)])

            if pre:
                pre(0)
                if post:
                    post(0)
                pre(1)
                if post:
                    post(1)
            stats(0)
            for tt in range(NT):
                if pre and tt + 2 < NT:
                    pre(tt + 2)
                if tt + 1 < NT:
                    stats(tt + 1)
                modt(tt)
                if post and tt + 2 < NT:
                    post(tt + 2)
                if tt >= 1:
                    copyt(tt - 1)
            copyt(NT - 1)

        norm_pipeline(lambda tt: xs[tt % 3], xk, modbuf[:, 1, :], modbuf[:, 0, :], "mod0_1", "mod0_0",
                      pre=lambda tt: S.dma("sp", xs[tt % 3], x[tt * 128:(tt + 1) * 128, :], writes=["xs%d" % (tt % 3)], semkey="xs%d" % (tt % 3)))
        mod_cols(2048, [modbuf[:, 2, :]], ["mod0_2"], 4)

        def hTq(tq):
            return [("hT", 4 * tq + i) for i in range(4)]

        if stop_after == "norm1":
            dtmp = V(16384 * 4, F32)
            S.op("dve", CP(dtmp, hT.rearrange("p k t -> p (k t)")), reads=[("hT", i) for i in range(NT)], writes=["dtmp"])
            S.dma("sp", dbg[:, :], dtmp, reads=["dtmp"], writes=["dbg"], semkey="dbg")
            S.wait_all("sp", ["dbg"])
            S.emit()
            return nc

        Ar.off = m0
        ALIAS0 = ["cbc", "g1bc", "xs0", "xs1", "xs2", "junk", "ntmp", "htm0", "htm1"]
        mixedT = V(8 * T * 2, BF16, "p (k t) -> p k t", t=T)
        v_hg = V(NT * 512 * 2, BF16, "p (n c) -> p n c", c=512)
        v_att_off = Ar.off
        v_att = V(NT * 768 * 2, BF16, "p (n c) -> p n c", c=768)
        m1 = Ar.off
        first_alias = [True]

        def alias_w():
            if first_alias[0]:
                first_alias[0] = False
                return list(ALIAS0)
            return []

        def load_w_cols(wi, segs):
            wv = wb[wi].rearrange("p (k c) -> p k c", c=512)
            o = 0
            for (c0, n) in segs:
                S.dma("pool", wv[:, :, o:o + n], w_in[:, c0:c0 + n].rearrange("(k p) c -> p k c", p=128),
                      writes=[WK[wi]], semkey=WK[wi])
                o += n
            return wv

        def v_segment_gen(c0, dst, dkey, grouped=False):
            wi = next_wb()
            wv = load_w_cols(wi, [(c0, 512)])
            for tt in range(NT):
                pb = 6 + (tt % 2)
                for kc in range(8):
                    S.op("pe", MM(ps[pb], hT[:, kc, tt * 128:(tt + 1) * 128], wv[:, kc, :], start=(kc == 0), stop=(kc == 7)),
                         reads=[("hT", tt), WK[wi]], writes=[PK[pb]], signal=(kc == 7))
                eng = "act" if tt % 2 == 0 else "dve"
                if grouped:
                    d_ap = dst[:, tt, :].rearrange("p (g a b) -> p g a b", g=4, a=3, b=64)[:, :, 0::2, :]
                    s_ap = ps[pb].rearrange("p (g a b) -> p g a b", g=4, a=2, b=64)
                else:
                    d_ap = dst[:, tt, :]
                    s_ap = ps[pb]
                fn = ACTF(d_ap, s_ap, AF.Copy) if eng == "act" else CP(d_ap, s_ap)
                S.op(eng, fn, reads=[PK[pb]], writes=[(dkey, tt)] + alias_w())
                yield

        def v_segment(c0, dst, dkey, grouped=False):
            for _ in v_segment_gen(c0, dst, dkey, grouped):
                pass

        v_segment(1024, v_hg, "vhg")

        mh = Ar.off
        QB = 512 * 4
        b3 = lambda ap: ap.rearrange("p (c j) -> p c j", j=64)

        def hg_set(n):
            d = {}
            for nm in ("a", "b", "c", "d", "e", "g"):
                d[nm] = V(QB, F32)
            for nm in ("kT", "khT", "qT", "qhT", "gs"):
                d[nm] = V(512 * 2, BF16)
            d["k_tm"] = V(512 * 2, BF16, "p (j k) -> p j k", k=128)
            d["attn"] = V(512 * 2, BF16, "p (j t) -> p j t", t=128)
            d["ab"] = V(16 * 4, F32)
            d["Sb"] = V(8 * 128 * 2, BF16, "p (c v) -> p c v", v=128)
            d["n"] = n
            return d

        HS = [hg_set(0), hg_set(1)]
        Shist = V(9 * 128 * 4, F32, "p (c v) -> p c v", v=128, off=v_att_off)
        osq = V(512 * 2, BF16, off=v_att_off + 1152)
        t_rs = V(QB, F32, off=v_att_off + 1152 + 256)
        t_o = V(QB, F32, off=v_att_off + 1152 + 256 + 512)
        for P in HS:
            S.op("dve", MS(P["attn"], 0.0), writes=["attn%d" % P["n"]])
        hw = {}

        def hg_s1(i):
            h, tq = i // 4, i % 4
            P = HS[i % 2]
            n = P["n"]
            K = lambda s: "%s%d" % (s, n)
            if tq == 0:
                wi = next_wb()
                hw[h] = (wi, load_w_cols(wi, [(h * 128, 128), (512 + h * 128, 128), (1536 + h * 128, 128)]))
            wi, wv = hw[h]
            cols = slice(tq * 512, (tq + 1) * 512)
            a, b, c, d, e = P["a"], P["b"], P["c"], P["d"], P["e"]
            g_ = P["g"]
            for seg in (1, 0, 2):
                for kc in range(8):
                    S.op("pe", MM(ps[seg], wv[:, kc, seg * 128:(seg + 1) * 128], hT[:, kc, cols], start=(kc == 0), stop=(kc == 7)),
                         reads=hTq(tq) + [WK[wi]], writes=[PK[seg]], signal=(kc == 7))
                    if kc % 4 == 3:
                        yield
            S.op("act", ACTF(a, ps[1], AF.Exp, scale=-1.0), reads=[PK[1]], writes=[K("a")])
            S.op("act", ACTF(e, ps[0], AF.Exp, scale=-1.0), reads=[PK[0]], writes=[K("e")])
            S.op("act", ACTF(g_, ps[2], AF.Exp, scale=-1.0), reads=[PK[2]], writes=[K("g")])
            yield
            S.op("act", ACTF(b, a, AF.Ln, bias=one1), reads=[K("a"), "one1"], writes=[K("b")])
            S.op("act", ACTF(e, e, AF.Ln, bias=one1), reads=[K("e"), "one1"], writes=[K("e")])
            S.op("act", ACTF(g_, g_, AF.Ln, bias=one1), reads=[K("g"), "one1"], writes=[K("g")])
            yield
            S.op("act", ACTF(b, b, AF.Exp, scale=-1.0), reads=[K("b")], writes=[K("b")])
            S.op("act", ACTF(e, e, AF.Exp, scale=-1.0), reads=[K("e")], writes=[K("e")])
            S.op("act", ACTF(g_, g_, AF.Exp, scale=-1.0), reads=[K("g")], writes=[K("g")])
            yield
            S.op("dve", STT(a, a, oml[:, h:h + 1], b, ALU.mult, ALU.mult), reads=[K("a"), K("b"), "oml"], writes=[K("a")])
            S.op("dve", TT(e, ps[0], e, ALU.mult), reads=[PK[0], K("e")], writes=[K("e")])
            S.op("dve", TT(P["gs"], ps[2], g_, ALU.mult), reads=[PK[2], K("g")], writes=[K("gs")])
            yield
            S.op("act", ACTF(b, b, AF.Ln, scale=oml[:, h:h + 1], bias=lb[:, h:h + 1]), reads=[K("b"), "oml", "lb"], writes=[K("b")])
            yield
            S.op("dve", lambda e_, b=b, c=c: e_.tensor_tensor_scan(out=c, data0=scanmask, data1=b, initial=0.0, op0=ALU.mult, op1=ALU.add),
                 reads=[K("b"), "cf"], writes=[K("c")])
            S.op("dve", TT(b3(b), b3(c), b3(c)[:, :, 31:32].to_broadcast([128, 8, 64]), ALU.subtract), reads=[K("c")], writes=[K("b")])
            yield
            S.op("act", ACTF(d, b, AF.Exp), reads=[K("b")], writes=[K("d")])
            S.op("act", ACTF(b, b, AF.Exp, scale=-1.0), reads=[K("b")], writes=[K("b")])
            S.op("act", ACTF(P["ab"][:, 0:8], b3(c)[:, :, 63], AF.Exp), reads=[K("c")], writes=[K("ab_a")])
            S.op("act", ACTF(P["ab"][:, 8:16], b3(c)[:, :, 31], AF.Exp), reads=[K("c")], writes=[K("ab_m")])
            yield

        def hg_s2(i):
            h, tq = i // 4, i % 4
            P = HS[i % 2]
            n = P["n"]
            K = lambda s: "%s%d" % (s, n)
            cols = slice(tq * 512, (tq + 1) * 512)
            a, b, c, d, e = P["a"], P["b"], P["c"], P["d"], P["e"]
            S.op("dve", TT(P["kT"], a, b, ALU.mult), reads=[K("a"), K("b")], writes=[K("kT")])
            S.op("dve", TT(b3(P["khT"]), b3(P["kT"]), b3(d)[:, :, 63:64].to_broadcast([128, 8, 64]), ALU.mult),
                 reads=[K("kT"), K("d")], writes=[K("khT")])
            yield
            for j in range(4):
                S.op("pe", TR(psb[7][:, j * 128:(j + 1) * 128], P["khT"][:, j * 128:(j + 1) * 128], ident),
                     reads=[K("khT"), "cb"], writes=[PK[7]], signal=(j == 3))
            S.op("dve", TT(P["qT"], e, d, ALU.mult), reads=[K("e"), K("d")], writes=[K("qT")])
            S.op("dve", TT(b3(P["qhT"]), b3(P["qT"]), P["ab"][:, 8:16].unsqueeze(2).to_broadcast([128, 8, 64]), ALU.mult),
                 reads=[K("qT"), K("ab_m")], writes=[K("qhT")])
            yield
            S.op("act", ACTF(P["k_tm"], psb[7][:, 0:512].rearrange("p (j k) -> p j k", k=128), AF.Copy),
                 reads=[PK[7]], writes=[K("k_tm")])
            for j in range(4):
                S.op("pe", MM(ps[5][:, j * 128:(j + 1) * 128], P["kT"][:, j * 128:(j + 1) * 128], P["qT"][:, j * 128:(j + 1) * 128]),
                     reads=[K("kT"), K("qT")], writes=[PK[5]], signal=(j == 3))
            yield
            for cch in range(8):
                p0 = 64 * (cch % 2)
                S.op("pe", MM(ps[3 + cch % 2][:, (cch // 2) * 128:(cch // 2 + 1) * 128],
                              P["k_tm"][p0:p0 + 64, cch // 2, :], v_hg[p0:p0 + 64, 4 * tq + cch // 2, h * 128:(h + 1) * 128]),
                     reads=[K("k_tm"), ("vhg", 4 * tq + cch // 2)], writes=[PK[3 + cch % 2]], signal=(cch >= 6))
            psA = ps[5].rearrange("p (j t) -> p j t", t=128)
            S.op("dve", STT(P["attn"][0:64, :, 0:64], psA[0:64, :, 0:64], 1.0e30, tri4[0:64], ALU.min, ALU.mult),
                 reads=[PK[5], "cb"], writes=[K("attn")])
            S.op("dve", STT(P["attn"][64:128, :, 64:128], psA[64:128, :, 64:128], 1.0e30, tri4[64:128], ALU.min, ALU.mult),
                 reads=[PK[5], "cb"], writes=[K("attn")])
            yield
            if tq == 0:
                S.op("dve", MS(Shist[:, 0, :], 0.0), writes=[("Sh", 0)])
            else:
                S.op("dve", CP(Shist[:, 0, :], Shist[:, 8, :]), reads=[("Sh", 8)], writes=[("Sh", 0)])
            for cch in range(8):
                S.op("dve", STT(Shist[:, cch + 1, :], Shist[:, cch, :], P["ab"][:, cch:cch + 1],
                                ps[3 + cch % 2][:, (cch // 2) * 128:(cch // 2 + 1) * 128], ALU.mult, ALU.add),
                     reads=[("Sh", cch), K("ab_a"), PK[3 + cch % 2]], writes=[("Sh", cch + 1)])
                if cch % 2 == 1:
                    yield
            S.op("act", ACTF(P["Sb"], Shist[:, 0:8, :], AF.Copy), reads=[("Sh", cch) for cch in range(8)], writes=[K("Sb")])
            yield
            for j in range(4):
                S.op("pe", MM(ps[6][:, j * 128:(j + 1) * 128], v_hg[:, 4 * tq + j, h * 128:(h + 1) * 128], P["attn"][:, j, :],
                              start=True, stop=False),
                     reads=[("vhg", 4 * tq + j), K("attn")], writes=[PK[6]], signal=False)
                for cc in range(2):
                    cch = 2 * j + cc
                    S.op("pe", MM(ps[6][:, j * 128 + cc * 64:j * 128 + cc * 64 + 64], P["Sb"][:, cch, :],
                                  P["qhT"][:, j * 128 + cc * 64:j * 128 + cc * 64 + 64], start=False, stop=(cc == 1)),
                         reads=[K("Sb"), K("qhT")], writes=[PK[6]], signal=(j == 3 and cc == 1))
            yield
            S.op("act", ACTF(osq, ps[6], AF.Square), reads=[PK[6]], writes=["osq"])
            yield
            S.op("pe", MM(ps[7], ones_bf, osq), reads=["osq", "cb"], writes=[PK[7]])
            yield
            S.op("act", ACTF(t_rs, ps[7], AF.Ln, scale=1.0 / 128, bias=epsb), reads=[PK[7], "eps"], writes=["t_rs"])
            yield
            S.op("act", ACTF(t_rs, t_rs, AF.Exp, scale=-0.5), reads=["t_rs"], writes=["t_rs"])
            yield
            S.op("dve", TT(t_o, ps[6], t_rs, ALU.mult), reads=[PK[6], "t_rs"], writes=["t_o"])
            S.op("dve", STT(mixedT[:, h, cols], t_o, hg_g, P["gs"], ALU.mult, ALU.mult), reads=["t_o", K("gs"), "cf"],
                 writes=[("mx", h, tq)])
            yield

        def zip_gens(g1, g2):
            l1 = list_steps = None
            d1 = d2 = False
            while not (d1 and d2):
                if not d1:
                    d1 = next(g1, "done") == "done"
                if not d2:
                    d2 = next(g2, "done") == "done"

        for _ in hg_s1(0):
            pass
        for i in range(16):
            if i + 1 < 16:
                zip_gens(hg_s1(i + 1), hg_s2(i))
            else:
                for _ in hg_s2(i):
                    pass

        if stop_after == "hgrn2":
            S.barrier(engs=("pe", "act", "dve", "sp", "pool"))
            S.dma("pool", dbg[:, 0:8192].rearrange("p (k t) -> p k t", t=T), mixedT[:, 0:4, :], reads=[], writes=["dbg"], semkey="dbg")
            S.wait_all("sp", ["dbg"])
            S.emit()
            return nc

        HG_KEYS = ["osq", "t_rs", "t_o", "Sb0", "Sb1"] + [("Sh", cch) for cch in range(9)]
        for n in range(2):
            HG_KEYS += ["%s%d" % (s, n) for s in ("a", "b", "c", "d", "e", "g", "kT", "khT", "qT", "qhT", "gs", "k_tm", "attn", "ab_a", "ab_m")]
        Ar.off = mh
        S.fence(("act", "dve", "pe"), ["osq", "t_rs", "t_o"] + [("Sh", cch) for cch in range(9)])
        S.op("dve", MS(v_att.rearrange("p n (g a b) -> p (n g) a b", g=4, a=3, b=64)[:, :, 1, :], 1.0), writes=["vones"])
        S.fence(("pe", "act", "dve", "sp"), HG_KEYS)
        vgen = v_segment_gen(3072, v_att, "vatt", grouped=True)
        QA = [[V(T * 2, BF16) for _ in range(2)] for _ in range(2)]
        KA = [[V(T * 2, BF16) for _ in range(2)] for _ in range(2)]
        t_sq = [V(512 * 2, BF16) for _ in range(2)]
        t_r = [V(QB, F32) for _ in range(2)]
        t_rd = V(QB, F32)
        PTb = [V(512 * 2, BF16) for _ in range(4)]
        kms = V(8 * 4, F32)
        kmT = [V(8 * 2, BF16) for _ in range(2)]
        g8 = V(8 * 8 * 4, F32, "p (j n) -> p j n", n=8)
        top8 = V(8 * 8 * 4, F32, "p (j n) -> p j n", n=8)
        stg = V(8 * 72 * 2, BF16, "p (j c) -> p j c", c=72)
        S.op("dve", MS(stg, 0.0), writes=["stg"])
        cbc2 = V(8 * 128 * 2, BF16, "p (k m) -> p k m", m=128)
        for kc in range(8):
            S.op("act", ACTF(cbc2[:, kc, :], ones_bf, AF.Copy, scale=cact[:, kc:kc + 1]), reads=["cb", "cact"], writes=["cbc2"])
        for par in range(2):
            for hh in range(2):
                S.op("dve", MS(QA[par][hh][64:128, :], 0.0), writes=[("qa", par, hh), ("qac", par, hh)])
                S.op("dve", MS(KA[par][hh][64:128, :], 0.0), writes=[("kac", par, hh)])
                S.dma("sp", KA[par][hh][64:76, :], kcd[:, :], writes=[("kac", par, hh)], semkey="kac%d%d" % (par, hh))

        def moba_side(hp):
            par = hp % 2
            wi = next_wb()
            wv = load_w_cols(wi, [(2048 + hp * 128, 128), (2560 + hp * 128, 128)])
            for hh in range(2):
                S.dma("sp", QA[par][hh][72:76, :], qcd[2 * hp + hh, :, :], writes=[("qac", par, hh)], semkey="qac%d%d" % (par, hh))
            for tq in range(4):
                cols = slice(tq * 512, (tq + 1) * 512)
                for seg in range(2):
                    for kc in range(8):
                        S.op("pe", MM(ps[seg], wv[:, kc, seg * 128:(seg + 1) * 128], hT[:, kc, cols], start=(kc == 0), stop=(kc == 7)),
                             reads=hTq(tq) + [WK[wi]], writes=[PK[seg]], signal=(kc == 7))
                        if kc % 2 == 1:
                            yield
                for seg in range(2):
                    S.op("act", ACTF(t_sq[seg], ps[seg], AF.Square), reads=[PK[seg]], writes=["t_sq%d" % seg])
                    yield
                for seg in range(2):
                    S.op("pe", MM(ps[2], bd_ones, t_sq[seg]), reads=["t_sq%d" % seg, "cb"], writes=[PK[2]])
                    yield
                    S.op("act", ACTF(t_r[seg], ps[2], AF.Ln, scale=1.0 / 64, bias=epsb), reads=[PK[2], "eps"], writes=["t_r%d" % seg])
                    yield
                for seg in range(2):
                    S.op("act", ACTF(t_r[seg], t_r[seg], AF.Exp, scale=-0.5), reads=["t_r%d" % seg], writes=["t_r%d" % seg])
                yield
                for seg in range(2):
                    dst = QA[par] if seg == 0 else KA[par]
                    dkey = "qa" if seg == 0 else "ka"
                    gain = gq8 if seg == 0 else gk
                    for hh in range(2):
                        pr = slice(64 * hh, 64 * hh + 64)
                        S.op("dve", STT(dst[hh][0:64, cols], ps[seg][pr, :], gain[pr, :], t_r[seg][pr, :], ALU.mult, ALU.mult),
                             reads=[PK[seg], "t_r%d" % seg, "cf", "gq8"], writes=[(dkey, par, hh)])
                yield
            for hh in range(2):
                qa, ka = QA[par][hh], KA[par][hh]
                S.op("dve", lambda e_, ka=ka: e_.tensor_reduce(out=kms[0:64, :], in_=ka[0:64, :].rearrange("p (n j) -> p n j", j=256),
                                                               axis=AX.X, op=ALU.add),
                     reads=[("ka", par, hh)], writes=["kms"])
                S.op("dve", TS(kmT[hh][0:64, :], kms[0:64, :], 1.0 / 256, None, ALU.mult), reads=["kms"], writes=[("kmT", hh)])
                yield
                for j in range(8):
                    tt = 8 + j
                    S.op("pe", MM(ps[2][:, j * 8:(j + 1) * 8], qa[0:64, tt * 128:(tt + 1) * 128], kmT[hh][0:64, :]),
                         reads=[("qa", par, hh), ("kmT", hh)], writes=[PK[2]], signal=(j == 7))
                yield
                S.op("dve", TT(g8, ps[2][:, 0:64].rearrange("p (j n) -> p j n", n=8), gmask, ALU.add), reads=[PK[2], "cf"], writes=["g8"])
                for j in range(8):
                    S.op("dve", lambda e_, j=j: e_.max(out=top8[:, j, :], in_=g8[:, j, :]), reads=["g8"], writes=["top8"])
                yield
                S.op("dve", TT(g8, g8, top8[:, :, 2:3].to_broadcast([128, 8, 8]), ALU.is_lt), reads=["g8", "top8"], writes=["g8"])
                S.op("dve", TT(stg[:, :, 64:72], g8, pastmask, ALU.mult), reads=["g8", "cf"], writes=["stg"])
                yield
                for j in range(8):
                    S.op("pe", TR(psb[2][0:72, j * 128:(j + 1) * 128], stg[:, j, :], ident), reads=["stg", "cb"], writes=[PK[2]],
                         signal=(j == 7))
                yield
                S.op("act", ACTF(qa[64:72, 1024:2048], psb[2][64:72, 0:1024], AF.Copy), reads=[PK[2]], writes=[("qa", par, hh)])
                yield

        SCB = [4, 5, 7]
        ITS = []
        for hp_ in range(4):
            for hh_ in range(2):
                for tq_ in range(4):
                    for j_ in range(4 * tq_ + 4):
                        ITS.append((hp_, hh_, tq_, j_, 4 * tq_ + 4))
        side_gen = {}
        side_done = set()

        def ensure_pair(hp):
            if hp in side_done:
                return
            g_ = side_gen.get(hp)
            if g_ is None:
                g_ = moba_side(hp)
            for _ in g_:
                pass
            side_done.add(hp)

        def qk(g):
            hp, hh, tq, j, nkt = ITS[g]
            ensure_pair(hp)
            par = hp % 2
            qa, ka = QA[par][hh], KA[par][hh]
            t0 = max(j, 4 * tq)
            ncol = (4 * tq + 4 - t0) * 128
            pbk = SCB[g % 3]
            S.op("pe", MM(ps[pbk][:, 0:ncol], ka[:, j * 128:(j + 1) * 128], qa[:, t0 * 128:t0 * 128 + ncol]),
                 reads=[("ka", par, hh), ("kac", par, hh), ("qa", par, hh), ("qac", par, hh)], writes=[PK[pbk]])

        side0 = moba_side(0)
        d1 = d2 = False
        while not (d1 and d2):
            if not d1:
                d1 = next(vgen, "done") == "done"
            for _ in range(4):
                if not d2:
                    d2 = next(side0, "done") == "done"
        side_done.add(0)

        qk(0)
        qk(1)
        pending = []
        qcount = 0
        for g in range(len(ITS)):
            hp, hh, tq, j, nkt = ITS[g]
            par = hp % 2
            h = 2 * hp + hh
            if hh == 0 and tq == 0 and j == 0 and hp + 1 < 4:
                side_gen[hp + 1] = moba_side(hp + 1)
            if hh == 0 and tq == 0 and j == 0 and hp == 3:
                def mod2_side():
                    for ci_, (dst_, key_) in enumerate(((modbuf[:, 0, :], "mod1_0"), (modbuf[:, 1, :], "mod1_1"), (gate2, "mod1_2"))):
                        for _ in mod_cols_gen(3072 + ci_ * D, [dst_], [key_], 0, cb_ap=cbc2, cb_key="cbc2"):
                            yield
                side_gen[4] = mod2_side()
            nump = slice(64 * hh, 64 * hh + 64)
            denp = slice(64 * (1 - hh), 64 * (1 - hh) + 64)
            vcol = hp * 192 + hh * 64
            t0 = max(j, 4 * tq)
            ncol = (4 * tq + 4 - t0) * 128
            lo = (t0 - 4 * tq) * 128
            pbk = SCB[g % 3]
            pt = PTb[g % 4]
            ptk = "PT%d" % (g % 4)
            bn = 6 if qcount % 2 == 0 else 3
            if g + 2 < len(ITS):
                qk(g + 2)
            S.op("act", ACTF(pt[:, 0:ncol], ps[pbk][:, 0:ncol], AF.Exp), reads=[PK[pbk]], writes=[ptk])
            if j >= 4 * tq:
                S.op("dve", TT(pt[:, 0:128], pt[:, 0:128], tri128, ALU.mult), reads=[ptk, "cb"], writes=[ptk])
            S.op("pe", MM(ps[bn][:, lo:lo + ncol], v_att[:, j, vcol:vcol + 128], pt[:, 0:ncol],
                          start=(j == 0), stop=(j == nkt - 1)),
                 reads=[("vatt", j), "vones", ptk], writes=[PK[bn]], signal=True)
            if j == nkt - 1:
                def norm(bn=bn, nump=nump, denp=denp, hp=hp, tq=tq, hh=hh):
                    S.op("act", ACTF(t_rd[nump, :], ps[bn][denp, :], AF.Ln), reads=[PK[bn]], writes=["t_rd"])
                    S.op("act", ACTF(t_rd[nump, :], t_rd[nump, :], AF.Exp, scale=-1.0), reads=["t_rd"], writes=["t_rd"])
                    S.op("dve", TT(mixedT[nump, 4 + hp, tq * 512:(tq + 1) * 512], ps[bn][nump, :], t_rd[nump, :], ALU.mult),
                         reads=[PK[bn], "t_rd"], writes=[("mx", 4 + hp, tq, hh)])
                pending.append((g + 3, norm))
                qcount += 1
            while pending and pending[0][0] <= g:
                pending.pop(0)[1]()
            sg = side_gen.get(hp + 1)
            if sg is not None and (hp + 1) not in side_done:
                if next(sg, "done") == "done":
                    side_done.add(hp + 1)
        for _, fn in pending:
            fn()
        if 4 in side_gen and 4 not in side_done:
            for _ in side_gen[4]:
                pass
            side_done.add(4)

        if stop_after == "moba":
            S.barrier(engs=("pe", "act", "dve", "sp", "pool"))
            S.dma("pool", dbg[:, 0:8192].rearrange("p (k t) -> p k t", t=T), mixedT[:, 4:8, :], reads=[], writes=["dbg"], semkey="dbg")
            S.wait_all("sp", ["dbg"])
            S.emit()
            return nc

        wis = []
        for hf in range(2):
            wi = next_wb()
            wv = wb[wi].rearrange("p (k c) -> p k c", c=512)
            S.dma("pool", wv, w_out[:, hf * 512:(hf + 1) * 512].rearrange("(k p) c -> p k c", p=128), writes=[WK[wi]], semkey=WK[wi])
            S.op("dve", TT(wv, wv, modbuf[:, 2, hf * 512:(hf + 1) * 512].unsqueeze(1).to_broadcast([128, 8, 512]), ALU.mult),
                 reads=[WK[wi], "mod0_2"], writes=[WK[wi]])
            wis.append((wi, wv))
        S.barrier()
        Ar.off = m0 + 8 * T * 2 // 4
        x1 = V(NT * D * 4, F32, "p (n d) -> p n d", d=D)
        m2 = Ar.off
        cbc_off = Ar.off
        cbc = V(8 * 128 * 2, BF16, "p (k m) -> p k m", m=128)
        g2bc_off = Ar.off
        g2bc = V(D * 4, F32)
        otmp_off = Ar.off
        otmp = [V(512 * 4, F32) for _ in range(2)]
        S.dma("sp", g2bc, norm2_g[0:1, :].partition_broadcast(128)[:, 0, :], writes=["g2bc"], semkey="g2bc")
        for tt in range(NT):
            S.dma("sp", x1[:, tt, :], x[tt * 128:(tt + 1) * 128, :], writes=[("x1", tt)], semkey="x1_%d" % tt)
        S.op("dve", STT(modbuf[:, 1, :], modbuf[:, 1, :], 1.0, g2bc, ALU.add, ALU.mult), reads=["mod1_1", "g2bc"], writes=["mod1_1"])
        junk = V(D * 2, BF16)
        ntmp = V(D * 4, F32, off=otmp_off)
        htm = [V(D * 2, BF16) for _ in range(2)]
        opit = [0]

        OPB = [2, 3, 4, 5]

        def outproj_pe(tt):
            for hf in range(2):
                wi, wv = wis[hf]
                pb = OPB[(2 * tt + hf) % 4]
                for kc in range(8):
                    S.op("pe", MM(ps[pb], mixedT[:, kc, tt * 128:(tt + 1) * 128], wv[:, kc, :], start=(kc == 0), stop=(kc == 7)),
                         reads=[WK[wi]], writes=[PK[pb]], signal=(kc == 7))

        def outproj_dve(tt):
            for hf in range(2):
                pb = OPB[(2 * tt + hf) % 4]
                S.op("dve", TT(x1[:, tt, hf * 512:(hf + 1) * 512], ps[pb], x1[:, tt, hf * 512:(hf + 1) * 512], ALU.add),
                     reads=[PK[pb], ("x1", tt)], writes=[("x1", tt)])

        norm_pipeline(lambda tt: x1[:, tt, :], lambda tt: [("x1", tt)], modbuf[:, 1, :], modbuf[:, 0, :], "mod1_1", "mod1_0",
                      pre=outproj_pe, post=outproj_dve)

        if stop_after == "x1":
            for tt in range(NT):
                S.dma("sp", out[tt * 128:(tt + 1) * 128, :], x1[:, tt, :], reads=[("x1", tt)], writes=["out"], semkey="out")
            S.wait_all("sp", ["out"])
            S.emit()
            return nc

        S.barrier(engs=("pe", "act", "dve"))
        Ar.off = m0
        aT = [V(4 * T * 2, BF16, "p (c t) -> p c t", t=T) for _ in range(2)]
        sgb = [V(512 * 2, BF16, off=cbc_off + i * 256) for i in range(2)]
        assert Ar.off <= m0 + 8 * T * 2 // 4
        assert otmp_off == g2bc_off + 1024
        wb.append(V(8 * 512 * 2, BF16, off=g2bc_off))
        wb.append(modbuf[:, 0:2, :].rearrange("p s d -> p (s d)").bitcast(BF16))
        WK.extend(["wb4", "wb5"])
        EXTRA_ALIAS = {4: ["g2bc", "ntmp", "otmp0", "otmp1"], 5: ["mod1_0", "mod1_1"]}
        fctr = [0]
        first_use = set()

        def next_fwb():
            i = fctr[0] % 6
            fctr[0] += 1
            extra = []
            if i in EXTRA_ALIAS and i not in first_use:
                first_use.add(i)
                extra = EXTRA_ALIAS[i]
            return i, extra

        groups = [(g * 4, 4) for g in range(5)] + [(20, 2)]
        for gi, (c0, nch) in enumerate(groups):
            a = aT[gi % 2]
            ak = "aT%d" % (gi % 2)
            wg_i, xg = next_fwb()
            wu_i, xu = next_fwb()
            wgv = wb[wg_i].rearrange("p (k c) -> p k c", c=512)
            wuv = wb[wu_i].rearrange("p (k c) -> p k c", c=512)
            ncol = nch * 128
            S.dma("pool", wgv[:, :, 0:ncol], w_gate[:, c0 * 128:c0 * 128 + ncol].rearrange("(k p) c -> p k c", p=128),
                  writes=[WK[wg_i]] + xg, semkey=WK[wg_i])
            S.dma("pool", wuv[:, :, 0:ncol], w_up[:, c0 * 128:c0 * 128 + ncol].rearrange("(k p) c -> p k c", p=128),
                  writes=[WK[wu_i]] + xu, semkey=WK[wu_i])
            wd_i, xd = next_fwb()
            wdv = wb[wd_i].rearrange("p (j c) -> p j c", c=1024)
            S.dma("pool", wdv[:, 0:nch, :], w_down[c0 * 128:(c0 + nch) * 128, :].rearrange("(j p) c -> p j c", p=128),
                  writes=[WK[wd_i]] + xd, semkey=WK[wd_i])
            S.op("dve", TT(wdv[:, 0:nch, :], wdv[:, 0:nch, :], gate2.unsqueeze(1).to_broadcast([128, nch, D]), ALU.mult),
                 reads=[WK[wd_i], "mod1_2"], writes=[WK[wd_i]])
            it = 0
            for ci in range(nch):
                for tq in range(4):
                    cols = slice(tq * 512, (tq + 1) * 512)
                    pg, pu = (0, 1) if it % 2 == 0 else (2, 3)
                    sg = sgb[it % 2]
                    sgk = "sg%d" % (it % 2)
                    it += 1
                    for kc in range(8):
                        S.op("pe", MM(ps[pg], wgv[:, kc, ci * 128:(ci + 1) * 128], hT[:, kc, cols], start=(kc == 0), stop=(kc == 7)),
                             reads=hTq(tq) + [WK[wg_i]], writes=[PK[pg]], signal=(kc == 7))
                    for kc in range(8):
                        S.op("pe", MM(ps[pu], wuv[:, kc, ci * 128:(ci + 1) * 128], hT[:, kc, cols], start=(kc == 0), stop=(kc == 7)),
                             reads=hTq(tq) + [WK[wu_i]], writes=[PK[pu]], signal=(kc == 7))
                    S.op("act", ACTF(sg, ps[pg], AF.Silu), reads=[PK[pg]], writes=[sgk])
                    S.op("dve", TT(a[:, ci, cols], sg, ps[pu], ALU.mult), reads=[sgk, PK[pu]], writes=[(ak, ci, tq)])
            it = 0
            for tt in range(NT):
                for hf in range(2):
                    pb = 4 + (it % 3)
                    ot = otmp[it % 2]
                    otk = "otmp%d" % (it % 2)
                    it += 1
                    for ci in range(nch):
                        S.op("pe", MM(ps[pb], a[:, ci, tt * 128:(tt + 1) * 128], wdv[:, ci, hf * 512:(hf + 1) * 512],
                                      start=(ci == 0), stop=(ci == nch - 1)),
                             reads=[(ak, ci, tt // 4), WK[wd_i]], writes=[PK[pb]], signal=(ci == nch - 1))
                    S.op("dve", TT(x1[:, tt, hf * 512:(hf + 1) * 512], ps[pb], x1[:, tt, hf * 512:(hf + 1) * 512], ALU.add),
                         reads=[PK[pb], ("x1", tt)], writes=[("x1", tt)])
                if gi == len(groups) - 1:
                    S.dma("sp", out[tt * 128:(tt + 1) * 128, :], x1[:, tt, :], reads=[("x1", tt)], writes=["out"], semkey="out")
        S.wait_all("sp", ["out"])
        S.emit()
    return nc


def _consts():
    bf = ml_dtypes.bfloat16
    cb = np.zeros((128, 768), np.float32)
    p = np.arange(128)
    cb[:, 0:128] = np.eye(128)
    cb[:, 128:256] = 1.0
    cb[:, 256:384] = (p[:, None] // 64 == p[None, :] // 64)
    cb[:, 384:512] = (p[:, None] <= p[None, :])
    t64 = np.arange(64)
    tri4 = ((p[:, None] % 64) <= t64[None, :]).astype(np.float32)
    cb[:, 512:768] = np.tile(tri4[:, None, :], (1, 4, 1)).reshape(128, 256)
    scan = np.ones((128, 512), np.float32)
    scan[:, 0::64] = 0.0
    t = np.arange(T)
    hi16 = (t // 16) * 16.0
    lo = (t % 16).astype(np.float32)
    kc = np.zeros((12, T), np.float32)
    for n in range(8):
        kc[n] = -BIG * (t // 256 == n)
    kc[8] = 1.0
    kc[9] = 1.0
    kc[10] = hi16
    kc[11] = lo
    qc = np.zeros((8, 4, T), np.float32)
    for h in range(8):
        sl = 2.0 ** (-(h + 1))
        qc[h, 0] = -sl * hi16
        qc[h, 1] = -sl * lo
        qc[h, 2] = sl
        qc[h, 3] = sl
    return cb.astype(bf), scan, kc.astype(bf), qc.astype(bf)


_CACHE = {}


def kernel(x, c, w_ada, b_ada, norm1_g, w_in, lb_logits, hg_norm_g, q_norm_g, k_norm_g,
           w_out, norm2_g, w_gate, w_up, w_down):
    f = lambda a: np.ascontiguousarray(np.asarray(a, dtype=np.float32))
    x = f(x); c = f(c)
    stop_after = DEBUG or None
    key = ("nc", stop_after)
    if key not in _CACHE:
        _CACHE[key] = build_program(stop_after)
    nc = _CACHE[key]
    cb, scan, kc, qc = _consts()
    lbl = f(lb_logits)
    shared = {
        "w_ada": f(w_ada)[0], "b_ada": f(b_ada), "norm1_g": f(norm1_g), "norm2_g": f(norm2_g),
        "w_in": f(w_in)[0], "w_out": f(w_out)[0], "w_gate": f(w_gate)[0], "w_up": f(w_up)[0], "w_down": f(w_down)[0],
        "cb": cb, "kconst": kc, "qconst": qc,
    }
    in_maps = []
    for b in range(8):
        cf = np.zeros((128, 672), np.float32)
        cf[:, 0:512] = scan
        for j in range(8):
            qb = (8 + j) // 2
            cf[:, 544 + j * 8 + qb:544 + (j + 1) * 8] = NEG
            cf[:, 608 + j * 8:608 + j * 8 + qb] = 1.0
        cf[:, 512:520] = c[b].reshape(8, 128).T
        cf[:, 520:524] = lbl[0].reshape(4, 128).T
        cf[:, 524:528] = lbl[1].reshape(4, 128).T
        cf[:, 528] = f(hg_norm_g)[0]
        cf[:, 529] = np.tile(f(q_norm_g)[0], 2)
        cf[:, 530] = np.tile(f(k_norm_g)[0], 2)
        m = dict(shared)
        m["x"] = x[b]
        m["cf"] = cf
        in_maps.append(m)
    res = run_bass_kernel_spmd(nc, in_maps, core_ids=list(range(8)))
    if DEBUG and DEBUG != "x1":
        return np.stack([r["dbg"] for r in res.results], axis=0)
    return np.stack([r["out"] for r in res.results], axis=0).astype(np.float32)
```
